# Optimizing a Trainium2 kernel written in Bass

```python
import math
import jax
import jax.numpy as jnp
from jax import lax
import numpy as np

D_MODEL = 1024
BATCH = 8
SEQ = 4096
DEPTH = 2

CTX_LEN = 256
GRID_W = 64
HEAD_DIM = 64
ROPE_THETA = 10000.0
Q_BLOCK = 128
EPS = 1e-6
A_HEADS = 8
A_KV_HEADS = 2
B_HEADS = 4
B_QK_DIM = 64
B_V_DIM = 128
C_HEADS = 4
C_DIM = 128
C_CHUNK = 64
CONV_W = 3
D_FF = 2816
N_EXPERTS = 8
TOP_K = 2
D_FF_EXPERT = 1408

IN_SPLITS = (A_HEADS * HEAD_DIM, A_KV_HEADS * HEAD_DIM, A_KV_HEADS * HEAD_DIM,
             B_HEADS * 2 * B_QK_DIM, B_HEADS * 2 * B_QK_DIM, B_HEADS * B_V_DIM,
             C_HEADS * C_DIM, C_HEADS * C_DIM, C_HEADS * C_DIM, C_HEADS * C_DIM, 4 * C_HEADS,
             3 * D_MODEL)
N_IN = sum(IN_SPLITS)

kernel_name = "hybrid_gqa_diffattn_mlstm_moe_dit"


def rms_norm(x, g):
    xf = x.astype(jnp.float32)
    y = xf * lax.rsqrt(jnp.mean(xf * xf, axis=-1, keepdims=True) + EPS)
    return (y * g.astype(jnp.float32)).astype(x.dtype)


def split_heads(x, n):
    b, t, _ = x.shape
    return x.reshape(b, t, n, -1).transpose(0, 2, 1, 3)


def merge_heads(x):
    b, h, t, d = x.shape
    return x.transpose(0, 2, 1, 3).reshape(b, t, h * d)


def split_columns(h):
    out, off = [], 0
    for w in IN_SPLITS:
        out.append(h[..., off:off + w])
        off += w
    return out


def axial_angles(n_tok, dim):
    rows = n_tok // GRID_W
    row = jnp.repeat(jnp.arange(rows, dtype=jnp.float32), GRID_W)
    col = (jnp.arange(n_tok) % GRID_W).astype(jnp.float32)
    n_freq = dim // 4
    inv = ROPE_THETA ** (-jnp.arange(n_freq, dtype=jnp.float32) / n_freq)
    return row[:, None] * inv, col[:, None] * inv


def rope_half(x, ang):
    nf = ang.shape[-1]
    x1, x2 = x[..., :nf], x[..., nf:]
    cs, sn = jnp.cos(ang).astype(x.dtype), jnp.sin(ang).astype(x.dtype)
    return jnp.concatenate([x1 * cs - x2 * sn, x2 * cs + x1 * sn], axis=-1)


def axial_rope(x, ang_r, ang_c):
    h = x.shape[-1] // 2
    return jnp.concatenate([rope_half(x[..., :h], ang_r), rope_half(x[..., h:], ang_c)], axis=-1)


def full_attention(q, k, v, scale):
    s = jnp.einsum('bgrqd,bgkd->bgrqk', q, k, preferred_element_type=jnp.float32) * scale
    p = jax.nn.softmax(s, axis=-1).astype(v.dtype)
    return jnp.einsum('bgrqk,bgkv->bgrqv', p, v)


def block_attention(q, k, v, kc, vc, scale):
    k_all = jnp.concatenate([kc, k], axis=2)
    v_all = jnp.concatenate([vc, v], axis=2)
    b, g, r, t, d = q.shape
    nb = t // Q_BLOCK
    qb = jnp.moveaxis(q.reshape(b, g, r, nb, Q_BLOCK, d), 3, 0)
    o = lax.map(lambda qq: full_attention(qq, k_all, v_all, scale), qb)
    return jnp.moveaxis(o, 0, 3).reshape(b, g, r, t, -1)


def gqa_mixer(pl, pc, q_g, k_g, angs, with_ctx):
    def prep(q, k, v):
        return (rms_norm(split_heads(q, A_HEADS), q_g), rms_norm(split_heads(k, A_KV_HEADS), k_g),
                split_heads(v, A_KV_HEADS))
    ql, kl, vl = prep(*pl)
    qc, kc, vc = prep(*pc)
    ql = axial_rope(ql, *angs)
    kl = axial_rope(kl, *angs)
    rep = A_HEADS // A_KV_HEADS

    def group(q):
        b, h, t, d = q.shape
        return q.reshape(b, A_KV_HEADS, rep, t, d)

    def ungroup(o):
        b, g, r, t, d = o.shape
        return merge_heads(o.reshape(b, g * r, t, d))
    scale = HEAD_DIM ** -0.5
    out_l = ungroup(block_attention(group(ql), kl, vl, kc, vc, scale))
    out_c = ungroup(full_attention(group(qc), kc, vc, scale)) if with_ctx else None
    return out_l, out_c


def diff_mixer(pl, pc, lam_vecs, sub_g, lam_init, angs, with_ctx):
    def prep(q, k, v):
        return (split_heads(q, 2 * B_HEADS), split_heads(k, 2 * B_HEADS),
                jnp.repeat(split_heads(v, B_HEADS), 2, axis=1))
    ql, kl, vl = prep(*pl)
    qc, kc, vc = prep(*pc)
    ql = axial_rope(ql, *angs)
    kl = axial_rope(kl, *angs)
    lq1, lk1, lq2, lk2 = [a.astype(jnp.float32) for a in lam_vecs]
    lam = jnp.exp(jnp.sum(lq1 * lk1)) - jnp.exp(jnp.sum(lq2 * lk2)) + lam_init
    scale = B_QK_DIM ** -0.5

    def combine(o):
        b, _, _, t, dv = o.shape
        o = o.reshape(b, B_HEADS, 2, t, dv).astype(jnp.float32)
        dif = o[:, :, 0] - lam * o[:, :, 1]
        dif = rms_norm(dif, sub_g) * (1.0 - lam_init)
        return merge_heads(dif.astype(vl.dtype))
    out_l = combine(block_attention(ql[:, :, None], kl, vl, kc, vc, scale))
    out_c = combine(full_attention(qc[:, :, None], kc, vc, scale)) if with_ctx else None
    return out_l, out_c


def centred_conv(x, w, b):
    k = w.shape[0]
    p = k // 2
    t = x.shape[1]
    xp = jnp.pad(x, ((0, 0), (p, k - 1 - p), (0, 0)))
    y = xp[:, 0:t] * w[0]
    for j in range(1, k):
        y = y + xp[:, j:j + t] * w[j]
    return y + b


def mlstm_chunk_scan(q, k, v, i_pre, logf, state):
    b, h, t, dk = q.shape
    dv = v.shape[-1]
    L = C_CHUNK
    nc = t // L

    def chunks(a):
        return jnp.moveaxis(a.reshape((b, h, nc, L) + a.shape[3:]), 2, 0)
    xs = (chunks(q), chunks(k), chunks(v), chunks(i_pre), chunks(logf))
    tri = jnp.tril(jnp.ones((L, L), dtype=bool))

    def step(carry, inp):
        C, n, m = carry
        qc, kc, vc, ic, fc = inp
        bcum = jnp.cumsum(fc, axis=-1)
        logD = bcum[..., :, None] - bcum[..., None, :] + ic[..., None, :]
        logD = jnp.where(tri, logD, -jnp.inf)
        inter = bcum + m[..., None]
        m_t = jnp.maximum(inter, jnp.max(logD, axis=-1))
        dmat = jnp.exp(logD - m_t[..., None])
        a_inter = jnp.exp(inter - m_t)
        s = jnp.einsum('bhtd,bhsd->bhts', qc, kc, preferred_element_type=jnp.float32) * dmat
        num = a_inter[..., None] * jnp.einsum('bhvd,bhtd->bhtv', C, qc) + jnp.einsum('bhts,bhsv->bhtv', s, vc)
        den = a_inter * jnp.einsum('bhd,bhtd->bht', n, qc) + jnp.sum(s, axis=-1)
        hc = num / jnp.maximum(jnp.abs(den), jnp.exp(-m_t))[..., None]
        b_last = bcum[..., -1]
        w_log = b_last[..., None] - bcum + ic
        m_new = jnp.maximum(b_last + m, jnp.max(w_log, axis=-1))
        decay = jnp.exp(b_last + m - m_new)
        ws = jnp.exp(w_log - m_new[..., None])
        C_new = decay[..., None, None] * C + jnp.einsum('bhs,bhsv,bhsd->bhvd', ws, vc, kc)
        n_new = decay[..., None] * n + jnp.einsum('bhs,bhsd->bhd', ws, kc)
        return (C_new, n_new, m_new), hc
    state, hs = lax.scan(step, state, xs)
    return jnp.moveaxis(hs, 0, 2).reshape(b, h, t, dv).astype(v.dtype), state


def mlstm_mixer(pl, pc, conv_w, conv_b, gate_b, norm_g, with_ctx):
    def prep(q_pre, k_pre, v, o_pre, g_pre):
        b, t, _ = v.shape
        qk = jax.nn.silu(centred_conv(jnp.concatenate([q_pre, k_pre], axis=-1), conv_w, conv_b))
        q = split_heads(qk[..., :C_HEADS * C_DIM], C_HEADS)
        k = split_heads(qk[..., C_HEADS * C_DIM:], C_HEADS) * (C_DIM ** -0.5)
        vh = split_heads(v, C_HEADS)
        g = (g_pre.astype(jnp.float32) + gate_b.astype(jnp.float32)).reshape(b, t, 4, C_HEADS)
        g = g.transpose(2, 0, 3, 1)
        fwd = (g[0], jax.nn.log_sigmoid(g[1]))
        bwd = (g[2], jax.nn.log_sigmoid(g[3]))
        return q, k, vh, o_pre, fwd, bwd
    ql, kl, vl, ol, fl, bl = prep(*pl)
    qc, kc, vc, oc, fc, bc = prep(*pc)

    def flip(a):
        return jnp.flip(a, axis=2)

    def run(q, k, v, gates, state, reverse):
        if reverse:
            hh, st = mlstm_chunk_scan(flip(q), flip(k), flip(v), flip(gates[0]), flip(gates[1]), state)
            return flip(hh), st
        return mlstm_chunk_scan(q, k, v, gates[0], gates[1], state)
    bsz = qc.shape[0]
    zero = (jnp.zeros((bsz, C_HEADS, C_DIM, C_DIM), jnp.float32),
            jnp.zeros((bsz, C_HEADS, C_DIM), jnp.float32),
            jnp.zeros((bsz, C_HEADS), jnp.float32))
    hcf, st_f = run(qc, kc, vc, fc, zero, False)
    hcb, st_b = run(qc, kc, vc, bc, zero, True)
    hlf, _ = run(ql, kl, vl, fl, st_f, False)
    hlb, _ = run(ql, kl, vl, bl, st_b, True)

    def out(hf, hb, o_pre):
        return merge_heads(rms_norm(hf + hb, norm_g)) * jax.nn.sigmoid(o_pre)
    out_l = out(hlf, hlb, ol)
    out_c = out(hcf, hcb, oc) if with_ctx else None
    return out_l, out_c


def token_mixer(xl, xc, angs, lam_init, with_ctx, w_in, q_g, k_g, lq1, lk1, lq2, lk2, diff_g,
                conv_w, conv_b, gate_b, mlstm_g, w_br_a, w_br_b, w_br_c, w_out):
    cl = split_columns(xl @ w_in)
    cc = split_columns(xc @ w_in)
    a_l, a_c = gqa_mixer(cl[0:3], cc[0:3], q_g, k_g, angs, with_ctx)
    b_l, b_c = diff_mixer(cl[3:6], cc[3:6], (lq1, lk1, lq2, lk2), diff_g, lam_init, angs, with_ctx)
    m_l, m_c = mlstm_mixer(cl[6:11], cc[6:11], conv_w, conv_b, gate_b, mlstm_g, with_ctx)

    def merge(a, b, m, gate_pre):
        g_a, g_b, g_m = jnp.split(jax.nn.sigmoid(gate_pre.astype(jnp.float32)).astype(a.dtype), 3, axis=-1)
        return (g_a * (a @ w_br_a) + g_b * (b @ w_br_b) + g_m * (m @ w_br_c)) @ w_out
    y_l = merge(a_l, b_l, m_l, cl[11])
    y_c = merge(a_c, b_c, m_c, cc[11]) if with_ctx else None
    return y_l, y_c


def swiglu(x, wg, wu, wd):
    return (jax.nn.silu(x @ wg) * (x @ wu)) @ wd


def moe_swiglu(x, w_r, b_r, wg, wu, wd):
    shp = x.shape
    xt = x.reshape(-1, shp[-1])
    logits = (xt @ w_r).astype(jnp.float32) + b_r.astype(jnp.float32)
    top_v, top_i = lax.top_k(logits, TOP_K)
    top_w = jax.nn.softmax(top_v, axis=-1)
    gates = jnp.sum(top_w[..., None] * jax.nn.one_hot(top_i, N_EXPERTS, dtype=jnp.float32), axis=1)
    out = jnp.zeros_like(xt)
    for e in range(N_EXPERTS):
        out = out + gates[:, e:e + 1].astype(xt.dtype) * swiglu(xt, wg[e], wu[e], wd[e])
    return out.reshape(shp)


def setup_inputs(seed: int = 0) -> dict:
    key = jax.random.key(seed)
    ks = iter(jax.random.split(key, 48))
    f32 = jnp.float32
    D = D_MODEL
    L = DEPTH
    nd = (DEPTH + 1) // 2
    nm = DEPTH // 2

    def nrm(shape, scale):
        return jax.random.normal(next(ks), shape, f32) * scale

    def gain(shape):
        return 1.0 + nrm(shape, 0.05)
    i_bias = nrm((L, 2, C_HEADS), 0.1)
    f_bias = jnp.linspace(3.0, 6.0, C_HEADS, dtype=f32)[None, None, :] + nrm((L, 2, C_HEADS), 0.1)
    gate_b = jnp.stack([i_bias[:, 0], f_bias[:, 0], i_bias[:, 1], f_bias[:, 1]], axis=1).reshape(L, 4 * C_HEADS)
    return {
        "x": nrm((BATCH, SEQ, D), 1.0),
        "c": nrm((BATCH, D), 1.0),
        "ctx": nrm((BATCH, CTX_LEN, D), 1.0),
        "c_ctx": nrm((D,), 1.0),
        "ada_w": nrm((L, D, 6 * D), 0.5 * D ** -0.5),
        "ada_b": nrm((L, 6 * D), 0.02),
        "pre_mix_g": gain((L, D)),
        "post_mix_g": gain((L, D)),
        "pre_ffn_g": gain((L, D)),
        "post_ffn_g": gain((L, D)),
        "w_in": nrm((L, D, N_IN), D ** -0.5),
        "q_norm_g": gain((L, HEAD_DIM)),
        "k_norm_g": gain((L, HEAD_DIM)),
        "lam_q1": nrm((L, B_QK_DIM), 0.1),
        "lam_k1": nrm((L, B_QK_DIM), 0.1),
        "lam_q2": nrm((L, B_QK_DIM), 0.1),
        "lam_k2": nrm((L, B_QK_DIM), 0.1),
        "diff_norm_g": gain((L, B_V_DIM)),
        "conv_w": nrm((L, CONV_W, 2 * C_HEADS * C_DIM), CONV_W ** -0.5),
        "conv_b": nrm((L, 2 * C_HEADS * C_DIM), 0.02),
        "mlstm_gate_b": gate_b,
        "mlstm_norm_g": gain((L, C_DIM)),
        "w_br_attn": nrm((L, A_HEADS * HEAD_DIM, D), (A_HEADS * HEAD_DIM) ** -0.5),
        "w_br_diff": nrm((L, B_HEADS * B_V_DIM, D), (B_HEADS * B_V_DIM) ** -0.5),
        "w_br_mlstm": nrm((L, C_HEADS * C_DIM, D), (C_HEADS * C_DIM) ** -0.5),
        "w_out": nrm((L, D, D), D ** -0.5),
        "w_ff_gate": nrm((nd, D, D_FF), D ** -0.5),
        "w_ff_up": nrm((nd, D, D_FF), D ** -0.5),
        "w_ff_down": nrm((nd, D_FF, D), D_FF ** -0.5),
        "w_router": nrm((nm, D, N_EXPERTS), D ** -0.5),
        "b_router": nrm((nm, N_EXPERTS), 0.01),
        "w_moe_gate": nrm((nm, N_EXPERTS, D, D_FF_EXPERT), D ** -0.5),
        "w_moe_up": nrm((nm, N_EXPERTS, D, D_FF_EXPERT), D ** -0.5),
        "w_moe_down": nrm((nm, N_EXPERTS, D_FF_EXPERT, D), D_FF_EXPERT ** -0.5),
    }


def reference(x, c, ctx, c_ctx, ada_w, ada_b, pre_mix_g, post_mix_g, pre_ffn_g, post_ffn_g, w_in,
              q_norm_g, k_norm_g, lam_q1, lam_k1, lam_q2, lam_k2, diff_norm_g, conv_w, conv_b,
              mlstm_gate_b, mlstm_norm_g, w_br_attn, w_br_diff, w_br_mlstm, w_out, w_ff_gate, w_ff_up,
              w_ff_down, w_router, b_router, w_moe_gate, w_moe_up, w_moe_down):
    n_tok = x.shape[1]
    angs = axial_angles(n_tok, HEAD_DIM)
    for l in range(DEPTH):
        last = l == DEPTH - 1
        lam_init = 0.8 - 0.6 * math.exp(-0.3 * l)
        mod = jax.nn.silu(c) @ ada_w[l] + ada_b[l]
        mod_c = jax.nn.silu(c_ctx) @ ada_w[l] + ada_b[l]
        sh1, sc1, g1, sh2, sc2, g2 = jnp.split(mod[:, None, :], 6, axis=-1)
        sh1c, sc1c, g1c, sh2c, sc2c, g2c = jnp.split(mod_c, 6)
        xn = rms_norm(x, pre_mix_g[l]) * (1.0 + sc1) + sh1
        cn = rms_norm(ctx, pre_mix_g[l]) * (1.0 + sc1c) + sh1c
        y, yc = token_mixer(xn, cn, angs, lam_init, not last, w_in[l], q_norm_g[l], k_norm_g[l],
                            lam_q1[l], lam_k1[l], lam_q2[l], lam_k2[l], diff_norm_g[l], conv_w[l], conv_b[l],
                            mlstm_gate_b[l], mlstm_norm_g[l], w_br_attn[l], w_br_diff[l], w_br_mlstm[l], w_out[l])
        x = x + g1 * rms_norm(y, post_mix_g[l])
        if not last:
            ctx = ctx + g1c * rms_norm(yc, post_mix_g[l])

        def channel(h):
            if l % 2 == 0:
                j = l // 2
                return swiglu(h, w_ff_gate[j], w_ff_up[j], w_ff_down[j])
            j = l // 2
            return moe_swiglu(h, w_router[j], b_router[j], w_moe_gate[j], w_moe_up[j], w_moe_down[j])
        xn = rms_norm(x, pre_ffn_g[l]) * (1.0 + sc2) + sh2
        x = x + g2 * rms_norm(channel(xn), post_ffn_g[l])
        if not last:
            cn = rms_norm(ctx, pre_ffn_g[l]) * (1.0 + sc2c) + sh2c
            ctx = ctx + g2c * rms_norm(channel(cn), post_ffn_g[l])
    return x
```

```python
import numpy as np
import concourse.bass as bass
import concourse.mybir as mybir
from concourse.bass_utils import run_bass_kernel_spmd

F32 = mybir.dt.float32
BF16 = mybir.dt.bfloat16
AF = mybir.ActivationFunctionType
ALU = mybir.AluOpType
AX = mybir.AxisListType


class Buf:
    __slots__ = ("name", "t", "w", "r")

    def __init__(self, name, t=None):
        self.name = name
        self.t = t
        self.w = None
        self.r = []


EPOCH = 20000
NDMA = 24


class Sched:
    def __init__(self, nc):
        self.nc = nc
        self.eng = {"pe": nc.tensor, "act": nc.scalar, "dve": nc.vector, "pool": nc.gpsimd, "sp": nc.sync}
        self.cnt = {e: 0 for e in self.eng}
        self.pending = {e: [] for e in self.eng}
        self.sems = {e: [] for e in self.eng}
        self.seen = {e: {} for e in self.eng}
        self.dma_sems = {}
        self.dma_cnt = {}
        self.dma_last = {}
        self._stack = []
        self.nwaits = 0
        self.nops = 0

    def _sem(self, name):
        g = self.nc.semaphore(name)
        s = g.__enter__()
        self._stack.append(g)
        return s

    def sb(self, name, shape, dtype):
        self._uid = getattr(self, "_uid", 0) + 1
        g = self.nc.sbuf_tensor("%s_%d" % (name, self._uid), shape, dtype)
        t = g.__enter__()
        (self._phase if self._phase is not None else self._stack).append(g)
        return Buf(name, t)

    _phase = None

    def phase_begin(self):
        assert self._phase is None
        self._phase = []

    def phase_end(self):
        self.barrier()
        for g in reversed(self._phase):
            g.__exit__(None, None, None)
        self._phase = None

    def ps(self, name, shape, dtype=F32):
        g = self.nc.psum_tensor(name, shape, dtype)
        t = g.__enter__()
        self._stack.append(g)
        return Buf(name, t)

    def _eng_sem(self, e, idx):
        ep = (idx - 1) // EPOCH
        while len(self.sems[e]) <= ep:
            self.sems[e].append(self._sem("s_%s_%d" % (e, len(self.sems[e]))))
        return self.sems[e][ep], (idx - 1) % EPOCH + 1, ep

    def _wait(self, e, tok):
        if tok is None:
            return
        if tok[0] == "dma":
            _, q, slot, val = tok
            key = ("dma", q, slot)
            if self.seen[e].get(key, 0) >= val:
                return
            self.seen[e][key] = val
            self.eng[e].wait_ge(self.dma_sems[(q, slot)], val)
            self.nwaits += 1
            return
        _, e2, ref = tok
        if e2 == e and e == "pe":
            return
        idx = ref[0]
        if idx is None:
            raise RuntimeError("dependency on un-marked op of engine %s" % e2)
        key = ("eng", e2)
        if self.seen[e].get(key, 0) >= idx:
            return
        self.seen[e][key] = idx
        sem, val, ep = self._eng_sem(e2, idx)
        self.eng[e].wait_ge(sem, val)
        self.nwaits += 1

    def _deps(self, e, reads, writes):
        for b in reads:
            self._wait(e, b.w)
        for b in writes:
            self._wait(e, b.w)
            for r in b.r:
                self._wait(e, r)

    def _commit(self, tok, reads, writes):
        for b in reads:
            b.r.append(tok)
            if len(b.r) > 64:
                b.r = b.r[-64:]
        for b in writes:
            b.w = tok
            b.r = []

    def op(self, e, fn, reads=(), writes=(), inc=True):
        self._deps(e, reads, writes)
        ins = fn()
        self.nops += 1
        ref = [None]
        tok = ("eng", e, ref)
        self.pending[e].append(ref)
        if inc:
            self.cnt[e] += 1
            idx = self.cnt[e]
            sem, val, ep = self._eng_sem(e, idx)
            ins.then_inc(sem, 1)
            for r in self.pending[e]:
                r[0] = idx
            self.pending[e] = []
        self._commit(tok, reads, writes)
        return tok

    def dma(self, q, out, in_, reads=(), writes=(), **kw):
        n = self.dma_cnt.get(q, 0)
        self.dma_cnt[q] = n + 1
        slot = n % NDMA
        if (q, slot) not in self.dma_sems:
            self.dma_sems[(q, slot)] = self._sem("d_%s_%d" % (q, slot))
        val = 16 * (n // NDMA + 1)
        if val > 16:
            self._wait(q, ("dma", q, slot, val - 16))
        self._deps(q, reads, writes)
        ins = self.eng[q].dma_start(out=out, in_=in_, **kw)
        ins.then_inc(self.dma_sems[(q, slot)], 16)
        self.nops += 1
        tok = ("dma", q, slot, val)
        self._commit(tok, reads, writes)
        self.dma_last.setdefault(q, []).append(tok)
        return tok

    def barrier(self):
        toks = []
        for e in ("pe", "act", "dve", "pool"):
            if self.cnt[e] > 0:
                if self.pending[e]:
                    raise RuntimeError("barrier with un-marked pending ops on %s" % e)
                toks.append(("eng", e, [self.cnt[e]]))
        for q, lst in self.dma_last.items():
            toks.extend(lst[-NDMA:])
        for e in self.eng:
            for t in toks:
                if t[0] == "eng" and t[1] == e:
                    continue
                self._wait(e, t)

    def finish(self):
        self.barrier()
        for g in reversed(self._stack):
            g.__exit__(None, None, None)
        self._stack = []


DM = 1024
KT = 8
EPS = 1e-6
N_IN = 7440
C_AQ, C_AK, C_AV, C_BQ, C_BK, C_BV, C_CQ, C_CK, C_CV, C_CO, C_CG, C_GATE = (
    0, 512, 640, 768, 1280, 1792, 2304, 2816, 3328, 3840, 4352, 4368)
D_FF = 2816
N_EXP = 8
D_FFE = 1408
DEPTH = 2


class Cfg:
    def __init__(self, T=4096, TC=256, debug=False, stop_after=None):
        self.T, self.TC, self.NT = T, TC, T + TC
        self.debug = debug
        self.stop_after = stop_after
        self.blocks = []
        u = 0
        while u < TC:
            n = min(512, TC - u)
            self.blocks.append((u, n, True))
            u += n
        while u < self.NT:
            n = min(512, self.NT - u)
            self.blocks.append((u, n, False))
            u += n


class Builder:
    def __init__(self, cfg):
        self.cfg = cfg
        nc = self.nc = bass.Bass("TRN2", target_bir_lowering=False)
        S = self.S = Sched(nc)
        T, TC, NT = cfg.T, cfg.TC, cfg.NT
        self.inputs = {}
        self.outputs = {}

        def din(name, shape, dt=F32):
            ap = nc.dram_tensor(name, list(shape), dt, kind="ExternalInput").ap()
            self.inputs[name] = ap
            return ap
        self.din = din
        self.x_in = din("x", [T, DM])
        self.ctx_in = din("ctx", [TC, DM])
        self.cT = din("cT", [128, KT])
        self.cctxT = din("cctxT", [128, KT])
        self.ropeC = din("ropeC", [T, 64])
        self.ropeS = din("ropeS", [T, 64])
        W = self.W = {}
        for nm, shp in (("ada_w", [DEPTH, DM, 6 * DM]), ("ada_b", [DEPTH, 6 * DM]), ("pre_mix_g", [DEPTH, DM]),
                        ("post_mix_g", [DEPTH, DM]), ("pre_ffn_g", [DEPTH, DM]), ("post_ffn_g", [DEPTH, DM]),
                        ("w_in", [DEPTH, DM, N_IN]), ("q_norm_g", [DEPTH, 64]), ("k_norm_g", [DEPTH, 64]),
                        ("lam_q1", [DEPTH, 64]), ("lam_k1", [DEPTH, 64]), ("lam_q2", [DEPTH, 64]),
                        ("lam_k2", [DEPTH, 64]), ("diff_norm_g", [DEPTH, 128]), ("conv_w", [DEPTH, 3, 1024]),
                        ("conv_b", [DEPTH, 1024]), ("mlstm_gate_b", [DEPTH, 16]), ("mlstm_norm_g", [DEPTH, 128]),
                        ("w_br_attn", [DEPTH, 512, DM]), ("w_br_diff", [DEPTH, 512, DM]),
                        ("w_br_mlstm", [DEPTH, 512, DM]), ("w_out", [DEPTH, DM, DM]),
                        ("w_ff_gate", [1, DM, D_FF]), ("w_ff_up", [1, DM, D_FF]), ("w_ff_down", [1, D_FF, DM]),
                        ("w_router", [1, DM, N_EXP]), ("b_router", [1, N_EXP]),
                        ("w_moe_gate", [1, N_EXP, DM, D_FFE]), ("w_moe_up", [1, N_EXP, DM, D_FFE]),
                        ("w_moe_down", [1, N_EXP, D_FFE, DM])):
            W[nm] = din(nm, shp)
        self.out = nc.dram_tensor("out", [T, DM], F32, kind="ExternalOutput").ap()
        self.outputs["out"] = self.out
        self.scr = {}
        self.xres = self.dscr("xres", [T, DM], F32)
        self.cres = self.dscr("cres", [TC, DM], F32)
        self.PS = S.ps("PS", [128, 4096], F32)
        self.bank = [Buf("bank%d" % i, self.PS.t[:, i * 512:(i + 1) * 512]) for i in range(8)]
        self.pb = self.bank[0:6]
        self.pt = [Buf("pt%d" % i, self.bank[6 + i].t.bitcast(BF16)) for i in range(2)]
        self._rr = {}
        self.ident_b = S.sb("ident_b", [128, 128], BF16)
        self.ident_f = S.sb("ident_f", [128, 128], F32)
        self.ones_f = S.sb("ones_f", [128, 128], F32)
        self.ones_b = S.sb("ones_b", [128, 128], BF16)
        self.identd = din("ident", [128, 128])
        self.mlmask = din("mlmask", [64, 2, 64])
        S.dma("sp", self.ident_f.t[:], self.identd, writes=[self.ident_f])
        S.dma("pool", self.ident_b.t[:], self.identd, writes=[self.ident_b])
        S.op("dve", lambda: nc.vector.memset(self.ones_f.t[:], 1.0), writes=[self.ones_f])
        S.op("dve", lambda: nc.vector.memset(self.ones_b.t[:], 1.0), writes=[self.ones_b])

    def dscr(self, name, shape, dt):
        if name in self.scr:
            return self.scr[name]
        kind = "ExternalOutput" if self.cfg.debug else "Internal"
        ap = self.nc.dram_tensor(name, list(shape), dt, kind=kind).ap()
        self.scr[name] = ap
        if self.cfg.debug:
            self.outputs[name] = ap
        return ap

    def rr(self, key, lst):
        i = self._rr.get(key, 0)
        self._rr[key] = i + 1
        return lst[i % len(lst)]

    def mm(self, out, lhsT, rhs, start, stop, reads, writes):
        nc = self.nc
        return self.S.op("pe", lambda: nc.tensor.matmul(out, lhsT, rhs, start=start, stop=stop), reads, writes, inc=stop)

    def tr(self, out, in_, ident, reads, writes, inc=True):
        nc = self.nc
        return self.S.op("pe", lambda: nc.tensor.transpose(out, in_, ident), reads, writes, inc=inc)

    def act(self, out, in_, func, reads, writes, **kw):
        nc = self.nc
        return self.S.op("act", lambda: nc.scalar.activation(out, in_, func, **kw), reads, writes)

    def tt(self, out, a, b, op, reads, writes, eng="dve"):
        e = self.nc.vector if eng == "dve" else self.nc.gpsimd
        return self.S.op(eng, lambda: e.tensor_tensor(out, a, b, op), reads, writes)

    def ts(self, out, a, s1, s2, op0, op1, reads, writes, eng="dve"):
        e = self.nc.vector if eng == "dve" else self.nc.gpsimd
        if op1 is None:
            return self.S.op(eng, lambda: e.tensor_scalar(out, a, s1, None, op0), reads, writes)
        return self.S.op(eng, lambda: e.tensor_scalar(out, a, s1, s2, op0, op1), reads, writes)

    def stt(self, out, in0, scalar, in1, op0, op1, reads, writes):
        nc = self.nc
        return self.S.op("dve", lambda: nc.vector.scalar_tensor_tensor(out, in0, scalar, in1, op0, op1), reads, writes)

    def cp(self, out, in_, reads, writes, eng="dve"):
        if eng == "act":
            return self.act(out, in_, AF.Copy, reads, writes)
        e = self.nc.vector if eng == "dve" else self.nc.gpsimd
        return self.S.op(eng, lambda: e.tensor_copy(out, in_), reads, writes)

    def recip(self, out, in_, reads, writes):
        nc = self.nc
        return self.S.op("dve", lambda: nc.vector.reciprocal(out, in_), reads, writes)

    def recip_fast(self, out, in_, reads, writes):
        nc = self.nc
        return self.S.op("dve", lambda: nc.vector.reciprocal_approx_fast(out, in_), reads, writes)

    def rstd(self, out, ss, n, reads_writes):
        b = reads_writes
        self.ts(out, ss, 1.0 / n, EPS, ALU.mult, ALU.add, [b], [b])
        self.act(out, out, AF.Sqrt, [b], [b])
        self.recip(out, out, [b], [b])

    def phase_mod(self):
        S, nc = self.S, self.nc
        self.mod = self.dscr("mod", [DEPTH, 2, 6 * DM], F32)
        S.phase_begin()
        cs2 = S.sb("cs2", [128, KT, 2], F32)
        for which, src in ((0, self.cT), (1, self.cctxT)):
            b = S.sb("cs%d" % which, [128, KT], F32)
            S.dma("sp", b.t[:], src, writes=[b])
            self.act(cs2.t[:, :, which], b.t[:], AF.Silu, [b], [cs2])
        wb = [S.sb("adaw%d" % i, [128, KT, 512], F32) for i in range(3)]
        bT = S.sb("adabT", [128, 48], F32)
        bR = S.sb("adabR", [48, 128], F32)
        mT_ = S.sb("modT", [128, 2, 48], F32)
        mR = S.sb("modR", [96, 128], F32)
        NTL = 6 * DM // 128
        for l in range(DEPTH):
            wv = self.W["ada_w"][l].rearrange("(kt p) n -> p kt n", p=128)
            S.dma("sp", bR.t[:], self.W["ada_b"][l].rearrange("(t p) -> t p", p=128), writes=[bR])
            psb = self.rr("pb", self.pb)
            self.tr(psb.t[:, 0:NTL], bR.t[:], self.ident_f.t[0:NTL, 0:NTL], [bR, self.ident_f], [psb])
            self.cp(bT.t[:], psb.t[:, 0:NTL], [psb], [bT], eng="dve")
            ps = self.rr("pb", self.pb)
            for ch in range(12):
                w = self.rr("adaw", wb)
                S.dma("sp", w.t[:], wv[:, :, ch * 512:(ch + 1) * 512], writes=[w])
                for nt in range(4):
                    t = ch * 4 + nt
                    for kt in range(KT):
                        self.mm(ps.t[:, t:2 * NTL:NTL], w.t[:, kt, nt * 128:(nt + 1) * 128], cs2.t[:, kt, :], kt == 0, kt == KT - 1,
                                [w, cs2], [ps])
            self.tt(mT_.t[:], ps.t[:, 0:2 * NTL].rearrange("p (w t) -> p w t", w=2), bT.t[:].unsqueeze(1).broadcast_to([128, 2, NTL]),
                    ALU.add, [ps, bT], [mT_])
            ps2 = self.rr("pb", self.pb)
            self.tr(ps2.t[0:2 * NTL, 0:128], mT_.t[:].rearrange("p w t -> p (w t)"), self.ident_f.t[:], [mT_, self.ident_f], [ps2])
            self.cp(mR.t[:], ps2.t[0:2 * NTL, 0:128], [ps2], [mR], eng="act")
            for which in range(2):
                S.dma("sp", self.mod[l, which].rearrange("(t p) -> t p", p=128), mR.t[which * NTL:(which + 1) * NTL, :], reads=[mR])
        S.phase_end()

    def rstd_stages(self, out, ss, n, b):
        return [lambda: self.ts(out, ss, 1.0 / n, EPS, ALU.mult, ALU.add, [b], [b]),
                lambda: self.act(out, out, AF.Sqrt, [b], [b]),
                lambda: self.recip(out, out, [b], [b])]

    def rms_mod_stages(self, xt, A_bc, SH_bc, out, tmp, st):
        sts = [lambda: self.act(tmp.t[:], xt.t[:], AF.Square, [xt], [tmp, st], accum_out=st.t[:, 0:1])]
        sts += self.rstd_stages(st.t[:, 1:2], st.t[:, 0:1], DM, st)
        if SH_bc is None:
            sts.append(lambda: self.stt(out.t[:], xt.t[:], st.t[:, 1:2], A_bc.t[:], ALU.mult, ALU.mult, [xt, st, A_bc], [out]))
        else:
            sts.append(lambda: self.stt(tmp.t[:], xt.t[:], st.t[:, 1:2], A_bc.t[:], ALU.mult, ALU.mult, [xt, st, A_bc], [tmp]))
            sts.append(lambda: self.tt(out.t[:], tmp.t[:], SH_bc.t[:], ALU.add, [tmp, SH_bc], [out]))
        return sts

    @staticmethod
    def emit_interleaved(jobs, G):
        for g0 in range(0, len(jobs), G):
            grp = jobs[g0:g0 + G]
            for k in range(max(len(j) for j in grp)):
                for j in grp:
                    if k < len(j):
                        j[k]()

    def load_bc(self, buf, src_row):
        self.S.dma("sp", buf.t[:], src_row.partition_broadcast(128), writes=[buf])

    def rms_mod(self, xt, A_bc, SH_bc, out, tmp, st):
        self.act(tmp.t[:], xt.t[:], AF.Square, [xt], [tmp, st], accum_out=st.t[:, 0:1])
        self.rstd(st.t[:, 1:2], st.t[:, 0:1], DM, st)
        if SH_bc is None:
            self.stt(out.t[:], xt.t[:], st.t[:, 1:2], A_bc.t[:], ALU.mult, ALU.mult, [xt, st, A_bc], [out])
        else:
            self.stt(tmp.t[:], xt.t[:], st.t[:, 1:2], A_bc.t[:], ALU.mult, ALU.mult, [xt, st, A_bc], [tmp])
            self.tt(out.t[:], tmp.t[:], SH_bc.t[:], ALU.add, [tmp, SH_bc], [out])

    def make_mod_tiles(self, l, gname, sc_idx, sh_idx, which, tag, gbuf=None):
        S = self.S
        A = S.sb("A_" + tag, [128, DM], F32)
        SH = S.sb("SH_" + tag, [128, DM], F32)
        g = gbuf if gbuf is not None else S.sb("g_" + tag, [128, DM], F32)
        self.load_bc(A, self.mod[l, which, sc_idx * DM:(sc_idx + 1) * DM])
        self.load_bc(SH, self.mod[l, which, sh_idx * DM:(sh_idx + 1) * DM])
        self.load_bc(g, self.W[gname][l])
        self.stt(A.t[:], A.t[:], 1.0, g.t[:], ALU.add, ALU.mult, [A, g], [A])
        return A, SH

    def x_src(self, l, u0):
        TC = self.cfg.TC
        if u0 < TC:
            return (self.ctx_in if l == 0 else self.cres)[u0:u0 + 128, :]
        t0 = u0 - TC
        return (self.x_in if l == 0 else self.xres)[t0:t0 + 128, :]

    def phase_A(self, l):
        S, nc, cfg = self.S, self.nc, self.cfg
        T, TC, NT = cfg.T, cfg.TC, cfg.NT
        d = self.dscr
        aqT = d("aqT", [512, NT], BF16); akT = d("akT", [128, NT], BF16); av = d("av", [NT, 128], BF16)
        bqT = d("bqT", [512, NT], BF16); bkT = d("bkT", [512, NT], BF16); bv = d("bv", [NT, 512], BF16)
        cqkT = d("cqkT", [1024, NT], F32); cv = d("cv", [NT, 512], BF16); cog = d("cog", [NT, 512], BF16)
        cg = d("cg", [16, NT], F32); gT = d("gT", [3072, NT], BF16)
        S.phase_begin()
        xnT = S.sb("xnT", [128, KT, NT], BF16)
        xnT_blk = {}
        for (u0, n, isc) in cfg.blocks:
            xnT_blk[u0] = Buf("xnT_%d" % u0)
        mods = {}
        gsh = S.sb("gsh", [128, DM], F32)
        for which in (1, 0):
            mods[which] = self.make_mod_tiles(l, "pre_mix_g", 1, 0, which, "a%d" % which, gbuf=gsh)
        xt = [S.sb("xt%d" % i, [128, DM], F32) for i in range(2)]
        tmpn = [S.sb("tmpn%d" % i, [128, DM], F32) for i in range(2)]
        xnb = [S.sb("xnb%d" % i, [128, DM], BF16) for i in range(2)]
        st = [S.sb("st%d" % i, [128, 2], F32) for i in range(2)]
        jobs = []
        for (u0, n, isc) in cfg.blocks:
            for j in range(n // 128):
                def job(u0=u0, u=u0 + j * 128, isc=isc):
                    x = self.rr("xt", xt); xb = self.rr("xnb", xnb); s_ = self.rr("st", st); tmp = self.rr("tmpn", tmpn)
                    pt = self.rr("pt", self.pt)
                    A, SH = mods[1 if isc else 0]
                    sts = [lambda: S.dma("sp", x.t[:], self.x_src(l, u), writes=[x])]
                    sts += self.rms_mod_stages(x, A, SH, xb, tmp, s_)

                    def trs():
                        for kt in range(KT):
                            self.tr(pt.t[:, kt * 128:(kt + 1) * 128], xb.t[:, kt * 128:(kt + 1) * 128], self.ident_b.t[:],
                                    [xb, self.ident_b], [pt], inc=(kt == KT - 1))
                    sts.append(trs)
                    sts.append(lambda: self.cp(xnT.t[:, :, u:u + 128], pt.t[:].rearrange("p (k t) -> p k t", k=KT), [pt], [xnT_blk[u0]],
                                               eng=("act" if (u // 128) % 2 else "dve")))
                    return sts
                jobs.append(job)
        for g0 in range(0, len(jobs), 2):
            self.emit_interleaved([jb() for jb in jobs[g0:g0 + 2]], 2)
        if cfg.debug:
            dbg = d("dbg_xnT", [128, KT, NT], BF16)
            S.dma("sp", dbg, xnT.t[:], reads=list(xnT_blk.values()))
        wv = self.W["w_in"][l].rearrange("(kt p) n -> p kt n", p=128)
        wbuf = [S.sb("wch%d" % i, [128, KT, 512], BF16) for i in range(2)]
        latent_tiles = T // 128
        ropeC = S.sb("ropeC", [128, latent_tiles, 64], F32)
        ropeS = S.sb("ropeS", [128, latent_tiles, 64], F32)
        S.dma("sp", ropeC.t[:], self.ropeC.rearrange("(n p) d -> p n d", p=128), writes=[ropeC])
        S.dma("sp", ropeS.t[:], self.ropeS.rearrange("(n p) d -> p n d", p=128), writes=[ropeS])
        gq = S.sb("gq", [128, 64], F32); gk = S.sb("gk", [128, 64], F32)
        self.load_bc(gq, self.W["q_norm_g"][l]); self.load_bc(gk, self.W["k_norm_g"][l])
        f1 = [S.sb("f1_%d" % i, [128, 512], F32) for i in range(3)]
        f2 = [S.sb("f2_%d" % i, [128, 512], F32) for i in range(3)]
        f3 = [S.sb("f3_%d" % i, [128, 512], F32) for i in range(3)]
        ssb = [S.sb("ssb%d" % i, [128, 8], F32) for i in range(3)]
        ob = [S.sb("ob%d" % i, [128, 512], BF16) for i in range(4)]
        of = [S.sb("of%d" % i, [128, 512], F32) for i in range(2)]
        stT = [S.sb("stT%d" % i, [128, 4, 512], BF16) for i in range(2)]

        def load_w(c0, ncol):
            w = self.rr("wch", wbuf)
            S.dma("pool", w.t[:, :, 0:ncol], wv[:, :, c0:c0 + ncol], writes=[w])
            return w

        def rope(src, dst, H, lt):
            t1 = self.rr("f2", f2); t2 = self.rr("f3", f3)
            n = H * 64
            sv = src.t[:, 0:n].rearrange("p (h b j r) -> p h b j r", h=H, b=2, j=2)
            Cb = ropeC.t[:, lt, :].unsqueeze(1).broadcast_to([128, H, 64])
            self.tt(t1.t[:, 0:n].rearrange("p (h d) -> p h d", h=H), src.t[:, 0:n].rearrange("p (h d) -> p h d", h=H),
                    Cb, ALU.mult, [src, ropeC], [t1])
            t2v = t2.t[:, 0:n].rearrange("p (h b j r) -> p h b j r", h=H, b=2, j=2)
            Sv = ropeS.t[:, lt, :].rearrange("p (b j r) -> p b j r", b=2, j=2)
            for b in range(2):
                for j in range(2):
                    self.tt(t2v[:, :, b, j, :], sv[:, :, b, 1 - j, :],
                            Sv[:, b, j, :].unsqueeze(1).broadcast_to([128, H, 16]), ALU.mult, [src, ropeS], [t2])
            self.tt(dst.t[:, 0:n], t1.t[:, 0:n], t2.t[:, 0:n], ALU.add, [t1, t2], [dst])

        def headnorm(ps, H, g_bc, dst):
            n = H * 64
            sq = self.rr("f2", f2); ss = self.rr("ssb", ssb)
            self.act(sq.t[:, 0:n], ps.t[:, 0:n], AF.Square, [ps], [sq])
            self.S.op("dve", lambda: nc.vector.tensor_reduce(ss.t[:, 0:H], sq.t[:, 0:n].rearrange("p (h d) -> p h d", h=H),
                                                           AX.X, ALU.add), [sq], [ss])
            self.rstd(ss.t[:, 0:H], ss.t[:, 0:H], 64, ss)
            self.tt(sq.t[:, 0:n].rearrange("p (h d) -> p h d", h=H), ps.t[:, 0:n].rearrange("p (h d) -> p h d", h=H),
                    ss.t[:, 0:H].unsqueeze(2).broadcast_to([128, H, 64]), ALU.mult, [ps, ss], [sq])
            self.tt(dst.t[:, 0:n].rearrange("p (h d) -> p h d", h=H), sq.t[:, 0:n].rearrange("p (h d) -> p h d", h=H),
                    g_bc.t[:].unsqueeze(1).broadcast_to([128, H, 64]), ALU.mult, [sq, g_bc], [dst])

        def proj_tok(c0, ncol, epi):
            w = load_w(c0, ncol)
            pend = []
            for (u0, n, isc) in cfg.blocks:
                for j in range(n // 128):
                    u = u0 + j * 128
                    ps = self.rr("pb", self.pb)
                    for kt in range(KT):
                        self.mm(ps.t[:, 0:ncol], xnT.t[:, kt, u:u + 128], w.t[:, kt, 0:ncol], kt == 0, kt == KT - 1,
                                [xnT_blk[u0], w], [ps])
                    while len(pend) > 1:
                        pend.pop(0)()
                    tail = epi(ps, u, isc, j, (u0, n))
                    if tail is not None:
                        pend.append(tail)
            while pend:
                pend.pop(0)()

        def epi_T(dst_dram, row0, H, norm_g, do_rope):
            ncol = H * 64
            state = {}

            def epi(ps, u, isc, j, blk):
                u0, n = blk
                if norm_g is not None:
                    src = self.rr("f1", f1)
                    headnorm(ps, H, norm_g, src)
                else:
                    src = ps
                o = self.rr("ob", ob)
                if do_rope and not isc:
                    if src is ps:
                        src = self.rr("f1", f1)
                        self.cp(src.t[:, 0:ncol], ps.t[:, 0:ncol], [ps], [src], eng="act")
                    rope(src, o, H, (u - TC) // 128)
                else:
                    self.cp(o.t[:, 0:ncol], src.t[:, 0:ncol], [src], [o], eng="act")
                def tail():
                    if j == 0:
                        state["st"] = self.rr("stT", stT)
                    stg = state["st"]
                    nb = ncol // 128
                    pt = self.rr("pt", self.pt)
                    for b in range(nb):
                        self.tr(pt.t[:, b * 128:(b + 1) * 128], o.t[:, b * 128:(b + 1) * 128], self.ident_b.t[:],
                                [o, self.ident_b], [pt], inc=(b == nb - 1))
                    self.cp(stg.t[:, 0:nb, j * 128:(j + 1) * 128], pt.t[:, 0:nb * 128].rearrange("p (b t) -> p b t", b=nb),
                            [pt], [stg], eng="dve")
                    if j == n // 128 - 1:
                        S.dma("sp", dst_dram[row0:row0 + ncol, u0:u0 + n].rearrange("(b p) t -> p b t", p=128),
                              stg.t[:, 0:nb, 0:n], reads=[stg])
                return tail
            return epi

        def epi_copy(dst_dram, col0, ncol, func=None):
            def epi(ps, u, isc, j, blk):
                o = self.rr("ob", ob)
                if func is None:
                    self.cp(o.t[:, 0:ncol], ps.t[:, 0:ncol], [ps], [o], eng="act")
                else:
                    self.act(o.t[:, 0:ncol], ps.t[:, 0:ncol], func, [ps], [o])
                S.dma("sp", dst_dram[u:u + 128, col0:col0 + ncol], o.t[:, 0:ncol], reads=[o])
            return epi

        def proj_feat(c0, ncol, dst_dram, row0, func, dt):
            w = load_w(c0, ncol)
            for m0 in range(0, ncol, 128):
                m = min(128, ncol - m0)
                for (u0, n, isc) in cfg.blocks:
                    ps = self.rr("pb", self.pb)
                    for kt in range(KT):
                        self.mm(ps.t[0:m, 0:n], w.t[:, kt, m0:m0 + m], xnT.t[:, kt, u0:u0 + n], kt == 0, kt == KT - 1,
                                [xnT_blk[u0], w], [ps])
                    o = self.rr("ob", ob) if dt == BF16 else self.rr("of", of)
                    self.act(o.t[0:m, 0:n], ps.t[0:m, 0:n], func, [ps], [o])
                    S.dma("sp", dst_dram[row0 + m0:row0 + m0 + m, u0:u0 + n], o.t[0:m, 0:n], reads=[o])

        proj_tok(C_AQ, 512, epi_T(aqT, 0, 8, gq, True))
        proj_tok(C_AK, 128, epi_T(akT, 0, 2, gk, True))
        proj_tok(C_AV, 128, epi_copy(av, 0, 128))
        proj_tok(C_BQ, 512, epi_T(bqT, 0, 8, None, True))
        proj_tok(C_BK, 512, epi_T(bkT, 0, 8, None, True))
        proj_tok(C_BV, 512, epi_copy(bv, 0, 512))
        proj_tok(C_CV, 512, epi_copy(cv, 0, 512))
        proj_tok(C_CO, 512, epi_copy(cog, 0, 512, AF.Sigmoid))
        proj_feat(C_CQ, 512, cqkT, 0, AF.Copy, F32)
        proj_feat(C_CK, 512, cqkT, 512, AF.Copy, F32)
        proj_feat(C_CG, 16, cg, 0, AF.Copy, F32)
        for i in range(6):
            proj_feat(C_GATE + i * 512, 512, gT, i * 512, AF.Sigmoid, BF16)
        S.phase_end()


    def phase_attn(self, l, kind):
        S, nc, cfg = self.S, self.nc, self.cfg
        T, TC, NT = cfg.T, cfg.TC, cfg.NT
        ntile = NT // 128
        d = self.dscr
        last = (l == DEPTH - 1)
        lam_init = 0.8 - 0.6 * float(np.exp(-0.3 * l))
        if kind == "a":
            qT, kT, v, oT = d("aqT", [512, NT], BF16), d("akT", [128, NT], BF16), d("av", [NT, 128], BF16), d("aT", [512, NT], BF16)
            groups = [[h] for h in range(8)]
        else:
            qT, kT, v, oT = d("bqT", [512, NT], BF16), d("bkT", [512, NT], BF16), d("bv", [NT, 512], BF16), d("bT", [512, NT], BF16)
            groups = [[2 * h, 2 * h + 1] for h in range(4)]
        S.phase_begin()
        if kind == "a":
            V = S.sb("Vx", [128, ntile, 2, 128], BF16)
            for kh in range(2):
                S.dma("sp", V.t[:, :, kh, 0:64], v[:, kh * 64:(kh + 1) * 64].rearrange("(n p) c -> p n c", p=128), writes=[V])
            S.op("dve", lambda: nc.vector.memset(V.t[:, :, :, 64:128], 1.0), writes=[V])
        else:
            V = S.sb("V", [128, ntile, 512], BF16)
            S.dma("sp", V.t[:], v.rearrange("(n p) c -> p n c", p=128), writes=[V])
        qb = [S.sb("qTh%d" % i, [128, NT], BF16) for i in range(4)]
        kb = [S.sb("kTh%d" % i, [128, NT], BF16) for i in range(4)]
        pT = [S.sb("pT%d" % i, [128, 2, 512], BF16) for i in range(4)]
        fin = [S.sb("fin%d" % i, [128, 512], F32) for i in range(2)]
        rec = [S.sb("rec%d" % i, [128, 512], F32) for i in range(2)]
        osb = [S.sb("osb%d" % i, [128, 512], F32) for i in range(4)]
        ost = [S.sb("ost%d" % i, [128, 512], BF16) for i in range(2)]
        nsp = 3 if kind == "a" else 2
        spair = [(self.PS.t[:, i * 1024:(i + 1) * 1024], self.bank[2 * i], self.bank[2 * i + 1]) for i in range(nsp)]
        if kind == "a":
            obank = [self.bank[6], self.bank[7]]
        else:
            obank = [self.bank[4], self.bank[5]]
            dbank = [self.bank[6], self.bank[7]]
            accs = [S.sb("dacc%d" % i, [128, 2, 512], F32) for i in range(2)]
            PE_EVERY = 3
        if kind == "b":
            lv = {}
            for nm in ("lam_q1", "lam_k1", "lam_q2", "lam_k2"):
                lv[nm] = S.sb(nm, [128, 64], F32)
                self.load_bc(lv[nm], self.W[nm][l])
            lt = S.sb("lamtmp", [128, 64], F32)
            ls = S.sb("lams", [128, 4], F32)
            for i, (a, b) in enumerate((("lam_q1", "lam_k1"), ("lam_q2", "lam_k2"))):
                self.tt(lt.t[:], lv[a].t[:], lv[b].t[:], ALU.mult, [lv[a], lv[b]], [lt])
                S.op("dve", lambda: nc.vector.tensor_reduce(ls.t[:, i:i + 1], lt.t[:], AX.X, ALU.add), [lt], [ls])
            self.act(ls.t[:, 0:2], ls.t[:, 0:2], AF.Exp, [ls], [ls])
            self.tt(ls.t[:, 2:3], ls.t[:, 1:2], ls.t[:, 0:1], ALU.subtract, [ls], [ls])
            self.ts(ls.t[:, 2:3], ls.t[:, 2:3], -lam_init, None, ALU.add, None, [ls], [ls])
            gsub = S.sb("gsub", [128, 1], F32)
            S.dma("sp", gsub.t[:], self.W["diff_norm_g"][l].rearrange("(p o) -> p o", o=1), writes=[gsub])
            self.ts(gsub.t[:], gsub.t[:], 1.0 - lam_init, None, ALU.mult, None, [gsub], [gsub])
            sqbs = [S.sb("sqb%d" % i, [128, 512], F32) for i in range(2)]
            difs = [S.sb("dif%d" % i, [128, 512], F32) for i in range(2)]
        scale = 64 ** -0.5
        LA = 2 if kind == "a" else 1
        items = []
        cur_k = {}
        for grp in groups:
            for (u0, n, isc) in cfg.blocks:
                if isc and last:
                    continue
                ktiles = list(range(TC // 128)) if isc else list(range(ntile))
                pairs = [ktiles[i:i + 2] for i in range(0, len(ktiles), 2)]
                for gi, hq in enumerate(grp):
                    items.append(dict(grp=grp, gi=gi, hq=hq, u0=u0, n=n, pairs=pairs, first_of_grp=(u0 == [b for b in cfg.blocks if not (b[2] and last)][0][0])))
        loaded = {}

        def get_qk(it):
            key = tuple(it["grp"])
            if key not in loaded:
                qs, ks = [], []
                for hq in it["grp"]:
                    q = self.rr("qTh", qb)
                    for hf_ in range(2):
                        S.dma("sp", q.t[hf_ * 64:(hf_ + 1) * 64, :], qT[hq * 64:(hq + 1) * 64, :], writes=[q])
                    qs.append(q)
                    krow = (hq // 4) if kind == "a" else hq
                    if kind == "a" and cur_k.get("row") == krow:
                        ks.append(cur_k["buf"])
                    else:
                        k = self.rr("kTh", kb)
                        for hf_ in range(2):
                            S.dma("sp", k.t[hf_ * 64:(hf_ + 1) * 64, :], kT[krow * 64:(krow + 1) * 64, :], writes=[k])
                        cur_k["row"] = krow; cur_k["buf"] = k
                        ks.append(k)
                loaded[key] = (qs, ks)
            qs, ks = loaded[key]
            return qs[it["gi"]], ks[it["gi"]]
        grp_first = {}
        for it in items:
            grp_first.setdefault(tuple(it["grp"]), it)
        grp_order = list(grp_first.keys())

        units = [(it, pi) for it in items for pi in range(len(it["pairs"]))]

        def smm(unit):
            it, pi = unit
            if "q" not in it:
                it["q"], it["k"] = get_qk(it)
                gi_ = grp_order.index(tuple(it["grp"]))
                if it is grp_first[tuple(it["grp"])] and gi_ + 1 < len(grp_order):
                    get_qk(grp_first[grp_order[gi_ + 1]])
            q, k, n, u0 = it["q"], it["k"], it["n"], it["u0"]
            sp = self.rr("spair", spair)
            for hi, kt in enumerate(it["pairs"][pi]):
                rs = slice(hi * 64, (hi + 1) * 64)
                self.mm(sp[1 + hi].t[:, 0:n], k.t[rs, kt * 128:(kt + 1) * 128], q.t[rs, u0:u0 + n], True, True, [k, q], [sp[1 + hi]])
            return sp

        outs = []

        deferred = []
        r2s = [S.sb("r2_%d" % i, [64, 512], F32) for i in range(2)] if kind == "a" else None

        def finalize(it, ui):
            n, u0, hq, ob = it["n"], it["u0"], it["hq"], it["ob"]
            r = self.rr("rec", rec)
            if kind == "a":
                f = self.rr("fin", fin)
                self.cp(f.t[:, 0:n], ob.t[:, 0:n], [ob], [f], eng="dve")
                self.recip(r.t[64:128, 0:n], f.t[64:128, 0:n], [f], [r])
                r2 = self.rr("r2", r2s)
                S.dma("sp", r2.t[0:64, 0:n], r.t[64:128, 0:n], reads=[r], writes=[r2])

                def st2():
                    o = self.rr("ost", ost)
                    self.tt(o.t[0:64, 0:n], f.t[0:64, 0:n], r2.t[0:64, 0:n], ALU.mult, [f, r2], [o])
                    S.dma("sp", oT[hq * 64:(hq + 1) * 64, u0:u0 + n], o.t[0:64, 0:n], reads=[o])
                deferred.append((ui + 4, st2))
                return
            db, acc = it["db"], it["acc"]
            terms = [(acc, 0), (acc, 1)] if it["usedD"] else []
            for ti, (a_, hh) in enumerate(terms):
                self.mm(db.t[:, 0:n], self.ones_f.t[:], a_.t[:, hh, 0:n], (not it["den_started"]) and ti == 0, ti == len(terms) - 1,
                        [self.ones_f, a_], [db])
            assert terms, "every item accumulates at least one pair on DVE"
            self.act(r.t[:, 0:n], db.t[:, 0:n], AF.Ln, [db], [r])
            self.act(r.t[:, 0:n], r.t[:, 0:n], AF.Exp, [r], [r], scale=-1.0)
            o = self.rr("osb", osb)
            self.tt(o.t[:, 0:n], ob.t[:, 0:n], r.t[:, 0:n], ALU.mult, [ob, r], [o])
            ob1 = db
            outs.append(o)
            if len(outs) == 2:
                h = hq // 2
                o1, o2 = outs
                del outs[:]
                dif = self.rr("dif", difs); sqb = self.rr("sqb", sqbs)
                self.stt(dif.t[:, 0:n], o2.t[:, 0:n], ls.t[:, 2:3], o1.t[:, 0:n], ALU.mult, ALU.add, [o1, o2, ls], [dif])
                self.tt(sqb.t[:, 0:n], dif.t[:, 0:n], dif.t[:, 0:n], ALU.mult, [dif], [sqb])
                sb_ = ob1

                def st2():
                    self.mm(sb_.t[:, 0:n], self.ones_f.t[:], sqb.t[:, 0:n], True, True, [self.ones_f, sqb], [sb_])
                    self.ts(sqb.t[:, 0:n], sb_.t[:, 0:n], 1.0 / 128, EPS, ALU.mult, ALU.add, [sb_], [sqb])

                def st3():
                    self.act(sqb.t[:, 0:n], sqb.t[:, 0:n], AF.Ln, [sqb], [sqb])
                    self.act(sqb.t[:, 0:n], sqb.t[:, 0:n], AF.Exp, [sqb], [sqb], scale=-0.5)
                    oo = self.rr("ost", ost)
                    self.stt(oo.t[:, 0:n], dif.t[:, 0:n], gsub.t[:, 0:1], sqb.t[:, 0:n], ALU.mult, ALU.mult, [dif, gsub, sqb], [oo])
                    S.dma("sp", oT[h * 128:(h + 1) * 128, u0:u0 + n], oo.t[:, 0:n], reads=[oo])
                deferred.append((ui + 5, st2))
                deferred.append((ui + 9, st3))

        def run_deferred(ui):
            while deferred and deferred[0][0] <= ui:
                deferred.pop(0)[1]()

        queue = [smm(u) for u in units[0:LA]]
        for ui, (it, pi) in enumerate(units):
            sp = queue.pop(0)
            if ui + LA < len(units):
                queue.append(smm(units[ui + LA]))
            n, hq, pair = it["n"], it["hq"], it["pairs"][pi]
            npairs = len(it["pairs"])
            if pi == 0:
                it["ob"] = self.rr("obank", obank)
            ob = it["ob"]
            p = self.rr("pT", pT)
            np_ = len(pair)
            spv = sp[0].rearrange("p (h w) -> p h w", h=2)
            self.act(p.t[:, 0:np_, 0:n], spv[:, 0:np_, 0:n], AF.Exp, [sp[1], sp[2]][0:np_], [p], scale=scale)
            for hi, kt in enumerate(pair):
                first = (pi == 0 and hi == 0)
                lastk = (pi == npairs - 1 and hi == np_ - 1)
                if kind == "a":
                    self.mm(ob.t[:, 0:n], V.t[:, kt, hq // 4, :], p.t[:, hi, 0:n], first, lastk, [V, p], [ob])
                else:
                    self.mm(ob.t[:, 0:n], V.t[:, kt, (hq // 2) * 128:(hq // 2 + 1) * 128], p.t[:, hi, 0:n], first, lastk, [V, p], [ob])
            if kind == "b":
                if pi == 0:
                    it["db"] = self.rr("dbank", dbank); it["acc"] = self.rr("dacc", accs)
                    it["usedD"] = False; it["den_started"] = False
                db, acc = it["db"], it["acc"]
                if pi % PE_EVERY == PE_EVERY - 1 and pi != npairs - 1:
                    for hi in range(np_):
                        self.mm(db.t[:, 0:n], self.ones_b.t[:], p.t[:, hi, 0:n], not it["den_started"], False, [self.ones_b, p], [db])
                        it["den_started"] = True
                else:
                    if not it["usedD"]:
                        if np_ < 2:
                            S.op("dve", lambda: nc.vector.memset(acc.t[:, 1, 0:n], 0.0), writes=[acc])
                        self.cp(acc.t[:, 0:np_, 0:n], p.t[:, 0:np_, 0:n], [p], [acc], eng="dve")
                    else:
                        self.tt(acc.t[:, 0:np_, 0:n], acc.t[:, 0:np_, 0:n], p.t[:, 0:np_, 0:n], ALU.add, [p, acc], [acc])
                    it["usedD"] = True
            if pi == npairs - 1:
                finalize(it, ui)
            deferred.sort(key=lambda t: t[0])
            run_deferred(ui)
        run_deferred(10 ** 9)
        S.phase_end()

    def phase_mlstm(self, l):
        S, nc, cfg = self.S, self.nc, self.cfg
        T, TC, NT = cfg.T, cfg.TC, cfg.NT
        ntile = NT // 128
        nch = NT // 64
        cch = TC // 64
        d = self.dscr
        last = (l == DEPTH - 1)
        cqkT = d("cqkT", [1024, NT], F32); cv = d("cv", [NT, 512], BF16); cog = d("cog", [NT, 512], BF16)
        cg = d("cg", [16, NT], F32)
        cqT = d("cqT", [1024, NT], BF16); ktok = d("ktok", [NT, 512], BF16)
        hf = d("hf", [NT, 512], F32); hb = d("hb", [NT, 512], F32); mT = d("mT", [512, NT], BF16)
        S.phase_begin()
        xr = [S.sb("cx%d" % i, [128, NT], F32) for i in range(2)]
        yr = S.sb("cy", [128, NT], F32)
        yb = [S.sb("cyb%d" % i, [128, NT], BF16) for i in range(2)]
        cw = [S.sb("cw%d" % i, [128, 4], F32) for i in range(2)]
        kst = [S.sb("kst%d" % i, [128, 128], BF16) for i in range(3)]
        for j in range(8):
            x = self.rr("cx", xr); w = self.rr("cw", cw); y = self.rr("cyb", yb)
            S.dma("sp", x.t[:], cqkT[j * 128:(j + 1) * 128, :], writes=[x])
            for k in range(3):
                S.dma("sp", w.t[:, k:k + 1], self.W["conv_w"][l, k, j * 128:(j + 1) * 128].rearrange("(p o) -> p o", o=1), writes=[w])
            S.dma("sp", w.t[:, 3:4], self.W["conv_b"][l, j * 128:(j + 1) * 128].rearrange("(p o) -> p o", o=1), writes=[w])
            self.ts(yr.t[:], x.t[:], w.t[:, 1:2], w.t[:, 3:4], ALU.mult, ALU.add, [x, w], [yr])
            for (s, e) in ((0, TC), (TC, NT)):
                self.stt(yr.t[:, s + 1:e], x.t[:, s:e - 1], w.t[:, 0:1], yr.t[:, s + 1:e], ALU.mult, ALU.add, [x, w, yr], [yr])
                self.stt(yr.t[:, s:e - 1], x.t[:, s + 1:e], w.t[:, 2:3], yr.t[:, s:e - 1], ALU.mult, ALU.add, [x, w, yr], [yr])
            self.act(y.t[:], yr.t[:], AF.Silu, [yr], [y])
            S.dma("sp", cqT[j * 128:(j + 1) * 128, :], y.t[:], reads=[y])
            if j >= 4:
                for i in range(ntile):
                    pt = self.rr("pt", self.pt)
                    self.tr(pt.t[:, 0:128], y.t[:, i * 128:(i + 1) * 128], self.ident_b.t[:], [y, self.ident_b], [pt])
                    ks = self.rr("kst", kst)
                    self.cp(ks.t[:], pt.t[:, 0:128], [pt], [ks], eng="dve")
                    S.dma("sp", ktok[i * 128:(i + 1) * 128, (j - 4) * 128:(j - 3) * 128], ks.t[:], reads=[ks])
        S.phase_end()
        if not hasattr(self, "_ml_scal"):
            self._ml_scal = S.sb("scal", [64, nch, 24], F32)
            self._ml_eFbc = S.sb("eFbc", [128, 8, nch], F32)
        scal, eFbc = self._ml_scal, self._ml_eFbc
        S.phase_begin()
        KS = float(np.log(128.0 ** -0.5))
        E = S.sb("Esel", [4, 4, 128], F32)
        for j in range(4):
            self.cp(E.t[:, j, :], self.ident_f.t[0:4, j:j + 1].to_broadcast([4, 128]), [self.ident_f], [E])
        m01 = S.sb("m01", [4, NT], F32)
        S.op("dve", lambda: nc.vector.memset(m01.t[:], 1.0), writes=[m01])
        S.op("dve", lambda: nc.vector.memset(m01.t[:].rearrange("p (c s) -> p c s", s=64)[:, :, 0:1], 0.0), writes=[m01])
        gb = S.sb("gbias", [4, 4], F32)
        for g4 in range(4):
            S.dma("sp", gb.t[:, g4:g4 + 1], self.W["mlstm_gate_b"][l, g4 * 4:(g4 + 1) * 4].rearrange("(p o) -> p o", o=1), writes=[gb])
        I_ = S.sb("gI", [4, NT], F32); L_ = S.sb("gL", [4, NT], F32); PL = S.sb("gPL", [4, NT], F32)
        CUM = S.sb("gCUM", [4, NT], F32); q3 = [S.sb("gq%d" % i, [4, NT], F32) for i in range(3)]
        eF = S.sb("geF", [4, nch], F32)
        for dr in range(2):
            S.dma("sp", I_.t[:], cg[dr * 8:dr * 8 + 4, :], writes=[I_])
            S.dma("sp", L_.t[:], cg[dr * 8 + 4:dr * 8 + 8, :], writes=[L_])
            self.ts(I_.t[:], I_.t[:], gb.t[:, 2 * dr:2 * dr + 1], None, ALU.add, None, [I_, gb], [I_])
            self.ts(L_.t[:], L_.t[:], gb.t[:, 2 * dr + 1:2 * dr + 2], None, ALU.add, None, [L_, gb], [L_])
            self.act(L_.t[:], L_.t[:], AF.Exp, [L_], [L_], scale=-1.0)
            self.act(L_.t[:], L_.t[:], AF.Ln, [L_], [L_], bias=1.0)
            S.op("dve", lambda: nc.vector.tensor_tensor_scan(PL.t[:], m01.t[:], L_.t[:], 0.0, ALU.mult, ALU.add), [m01, L_], [PL])
            PLv = PL.t[:].rearrange("p (c s) -> p c s", s=64)
            PLlast = PLv[:, :, 63:64].broadcast_to([4, nch, 64])
            if dr == 0:
                self.cp(CUM.t[:], PL.t[:], [PL], [CUM])
            else:
                self.tt(CUM.t[:], L_.t[:], PL.t[:], ALU.subtract, [L_, PL], [CUM])
                self.tt(CUM.t[:].rearrange("p (c s) -> p c s", s=64), CUM.t[:].rearrange("p (c s) -> p c s", s=64), PLlast,
                        ALU.add, [CUM, PL], [CUM])
            self.tt(q3[0].t[:], I_.t[:], CUM.t[:], ALU.add, [I_, CUM], [q3[0]])
            self.tt(q3[1].t[:].rearrange("p (c s) -> p c s", s=64), q3[0].t[:].rearrange("p (c s) -> p c s", s=64), PLlast,
                    ALU.subtract, [q3[0], PL], [q3[1]])
            kb_ = S.sb("ksb%d" % dr, [4, 1], F32)
            S.op("dve", lambda: nc.vector.memset(kb_.t[:], KS), writes=[kb_])
            self.act(q3[0].t[:], q3[0].t[:], AF.Exp, [q3[0], kb_], [q3[0]], bias=kb_.t[:, 0:1])
            self.act(q3[1].t[:], q3[1].t[:], AF.Exp, [q3[1], kb_], [q3[1]], bias=kb_.t[:, 0:1])
            self.act(q3[2].t[:], CUM.t[:], AF.Exp, [CUM], [q3[2]])
            self.act(eF.t[:], PLv[:, :, 63], AF.Exp, [PL], [eF], scale=-1.0)
            for c in range(nch):
                ps = self.rr("pb", self.pb)
                for q in range(3):
                    self.mm(ps.t[0:64, q * 4:(q + 1) * 4], q3[q].t[:, c * 64:(c + 1) * 64], self.ident_f.t[0:4, 0:4], True, True,
                            [q3[q], self.ident_f], [ps])
                self.cp(scal.t[:, c, dr * 12:(dr + 1) * 12], ps.t[0:64, 0:12], [ps], [scal], eng=("act" if c % 2 else "dve"))
            for j in range(4):
                ps = self.rr("pb", self.pb)
                self.mm(ps.t[:, 0:nch], E.t[:, j, :], eF.t[:], True, True, [E, eF], [ps])
                self.cp(eFbc.t[:, dr * 4 + j, :], ps.t[:, 0:nch], [ps], [eFbc], eng="act")
        if cfg.debug:
            S.dma("sp", d("dbg_scal", [64, nch, 24], F32), scal.t[:], reads=[scal])
            S.dma("sp", d("dbg_eF", [128, 8, nch], F32), eFbc.t[:], reads=[eFbc])
        S.phase_end()
        S.phase_begin()
        maskt = S.sb("maskt", [64, 2, 64], F32)
        S.dma("sp", maskt.t[:], self.mlmask, writes=[maskt])
        order = {0: list(range(nch)), 1: list(range(cch - 1, -1, -1)) + list(range(nch - 1, cch - 1, -1))}
        qTb = [S.sb("mqT%d" % i, [128, NT], BF16) for i in range(2)]
        kTb = [S.sb("mkT%d" % i, [128, NT], BF16) for i in range(2)]
        kTokb = [S.sb("mktok%d" % i, [64, nch, 128], BF16) for i in range(2)]
        Vxb = [S.sb("mvx%d" % i, [64, nch, 129], BF16) for i in range(2)]
        st_f = [S.sb("mst%d" % i, [128, 129], F32) for i in range(4)]
        st_b = [S.sb("mstb%d" % i, [128, 129], BF16) for i in range(4)]
        Sm2 = [[S.sb("mSm%d_%d" % (i, k), [64, 64], BF16) for k in range(2)] for i in range(4)]
        Vwb = [S.sb("mVw%d" % i, [64, nch, 129], BF16) for i in range(4)]
        dn = [S.sb("mdn%d" % i, [64, 2], F32) for i in range(4)]
        hst = [[S.sb("mh%d_%d" % (i, k), [64, 128], F32) for k in range(2)] for i in range(4)]
        for hp in range(2):
            heads = (2 * hp, 2 * hp + 1)
            for i, j in enumerate(heads):
                S.dma("sp", qTb[i].t[:], cqT[j * 128:(j + 1) * 128, :], writes=[qTb[i]])
                S.dma("sp", kTb[i].t[:], cqT[512 + j * 128:512 + (j + 1) * 128, :], writes=[kTb[i]])
                S.dma("sp", kTokb[i].t[:], ktok[:, j * 128:(j + 1) * 128].rearrange("(c p) d -> p c d", p=64), writes=[kTokb[i]])
                S.dma("sp", Vxb[i].t[:, :, 0:128], cv[:, j * 128:(j + 1) * 128].rearrange("(c p) d -> p c d", p=64), writes=[Vxb[i]])
                S.op("dve", lambda: nc.vector.memset(Vxb[i].t[:, :, 128:129], 1.0), writes=[Vxb[i]])
            chains = [(i, j, dr) for i, j in enumerate(heads) for dr in range(2)]
            for ci, (i, j, dr) in enumerate(chains):
                cw_ = dr * 12 + 4 + j
                self.tt(Vwb[ci].t[:], Vxb[i].t[:], scal.t[:, :, cw_:cw_ + 1].broadcast_to([64, nch, 129]), ALU.mult,
                        [Vxb[i], scal], [Vwb[ci]])
                S.op("dve", lambda: nc.vector.memset(st_f[ci].t[:], 0.0), writes=[st_f[ci]])
                S.op("dve", lambda: nc.vector.memset(st_b[ci].t[:], 0.0), writes=[st_b[ci]])
            ubank = [self.bank[6], self.bank[7]]

            def step_info(step):
                out_ = []
                for ci, (i, j, dr) in enumerate(chains):
                    c = order[dr][step]
                    out_.append((ci, i, j, dr, c, slice(c * 64, (c + 1) * 64), dr * 12 + j))
                return out_

            def scores(step):
                sbk = self.bank[step % 2]
                for (ci, i, j, dr, c, cs, cb) in step_info(step):
                    self.mm(sbk.t[0:64, ci * 64:(ci + 1) * 64], kTb[i].t[:, cs], qTb[i].t[:, cs], True, True, [kTb[i], qTb[i]], [sbk])
                for (ci, i, j, dr, c, cs, cb) in step_info(step):
                    sm = Sm2[ci][step % 2]
                    self.stt(sm.t[:], sbk.t[0:64, ci * 64:(ci + 1) * 64], scal.t[:, c, cb:cb + 1], maskt.t[:, dr, :], ALU.mult, ALU.mult,
                             [sbk, scal, maskt], [sm])
            scores(0)
            for step in range(nch):
                info = step_info(step)
                Sm = [Sm2[ci][step % 2] for ci in range(4)]
                if step + 1 < nch:
                    scores(step + 1)
                for (ci, i, j, dr, c, cs, cb) in info:
                    ps_o = self.bank[2 + ci]
                    self.mm(ps_o.t[0:64, 0:129], qTb[i].t[:, cs], st_b[ci].t[:], True, False, [qTb[i], st_b[ci]], [ps_o])
                    self.mm(ps_o.t[0:64, 0:129], Sm[ci].t[:], Vxb[i].t[:, c, :], False, True, [Sm[ci], Vxb[i]], [ps_o])
                for (ci, i, j, dr, c, cs, cb) in info:
                    ub = ubank[ci // 2]
                    uo = (ci % 2) * 256
                    self.mm(ub.t[:, uo:uo + 129], kTokb[i].t[:, c, :], Vwb[ci].t[:, c, :], True, True, [kTokb[i], Vwb[ci]], [ub])
                for (ci, i, j, dr, c, cs, cb) in info:
                    ub = ubank[ci // 2]
                    uo = (ci % 2) * 256
                    self.stt(st_f[ci].t[:], st_f[ci].t[:], eFbc.t[:, dr * 4 + j, c:c + 1], ub.t[:, uo:uo + 129], ALU.mult, ALU.add,
                             [st_f[ci], eFbc, ub], [st_f[ci]])
                for (ci, i, j, dr, c, cs, cb) in info:
                    self.ts(dn[ci].t[:, 0:1], self.bank[2 + ci].t[0:64, 128:129], -1.0, scal.t[:, c, cb + 8:cb + 9], ALU.mult, ALU.max,
                            [self.bank[2 + ci], scal], [dn[ci]])
                for (ci, i, j, dr, c, cs, cb) in info:
                    self.tt(dn[ci].t[:, 0:1], dn[ci].t[:, 0:1], self.bank[2 + ci].t[0:64, 128:129], ALU.max, [dn[ci], self.bank[2 + ci]], [dn[ci]])
                for (ci, i, j, dr, c, cs, cb) in info:
                    self.recip(dn[ci].t[:, 1:2], dn[ci].t[:, 0:1], [dn[ci]], [dn[ci]])
                for (ci, i, j, dr, c, cs, cb) in info:
                    self.cp(st_b[ci].t[:], st_f[ci].t[:], [st_f[ci]], [st_b[ci]], eng="act")
                for (ci, i, j, dr, c, cs, cb) in info:
                    ps_o = self.bank[2 + ci]
                    h = self.rr("mh%d" % ci, hst[ci])
                    self.act(h.t[:], ps_o.t[0:64, 0:128], AF.Copy, [ps_o, dn[ci]], [h], scale=dn[ci].t[:, 1:2])
                    if not (last and c < cch):
                        S.dma("sp", (hf if dr == 0 else hb)[cs, j * 128:(j + 1) * 128], h.t[:], reads=[h])
        S.phase_end()
        S.phase_begin()
        ng = S.sb("mng", [128, 128], F32)
        self.load_bc(ng, self.W["mlstm_norm_g"][l])
        ha = [S.sb("mha%d" % i, [128, 512], F32) for i in range(2)]
        hb_ = [S.sb("mhb%d" % i, [128, 512], F32) for i in range(2)]
        og = [S.sb("mog%d" % i, [128, 512], BF16) for i in range(2)]
        sqs = [S.sb("msq%d" % i, [128, 512], F32) for i in range(2)]
        ss = [S.sb("mss%d" % i, [128, 4], F32) for i in range(2)]
        mo = [S.sb("mmo%d" % i, [128, 512], BF16) for i in range(2)]
        stT = [S.sb("mstT%d" % i, [128, 4, 512], BF16) for i in range(2)]
        for (u0, n, isc) in cfg.blocks:
            if isc and last:
                continue
            stg = self.rr("mstT", stT)
            jobs = []
            for jt in range(n // 128):
                def job(jt=jt, u=u0 + jt * 128, stg=stg):
                    a = self.rr("mha", ha); b = self.rr("mhb", hb_); o = self.rr("mog", og); s_ = self.rr("mss", ss); m = self.rr("mmo", mo)
                    sq = self.rr("msq", sqs); pt = self.rr("pt", self.pt)
                    av3 = a.t[:].rearrange("p (h d) -> p h d", h=4)

                    def loads():
                        S.dma("sp", a.t[:], hf[u:u + 128, :], writes=[a])
                        S.dma("sp", b.t[:], hb[u:u + 128, :], writes=[b])
                        S.dma("sp", o.t[:], cog[u:u + 128, :], writes=[o])

                    def trs():
                        for b4 in range(4):
                            self.tr(pt.t[:, b4 * 128:(b4 + 1) * 128], m.t[:, b4 * 128:(b4 + 1) * 128], self.ident_b.t[:], [m, self.ident_b], [pt],
                                    inc=(b4 == 3))
                    sts = [loads,
                           lambda: self.tt(a.t[:], a.t[:], b.t[:], ALU.add, [a, b], [a]),
                           lambda: self.act(sq.t[:], a.t[:], AF.Square, [a], [sq]),
                           lambda: S.op("dve", lambda: nc.vector.tensor_reduce(s_.t[:], sq.t[:].rearrange("p (h d) -> p h d", h=4), AX.X, ALU.add), [sq], [s_])]
                    sts += self.rstd_stages(s_.t[:], s_.t[:], 128, s_)
                    sts += [lambda: self.tt(av3, av3, s_.t[:].unsqueeze(2).broadcast_to([128, 4, 128]), ALU.mult, [a, s_], [a]),
                            lambda: self.tt(av3, av3, ng.t[:].unsqueeze(1).broadcast_to([128, 4, 128]), ALU.mult, [a, ng], [a]),
                            lambda: self.tt(m.t[:], a.t[:], o.t[:], ALU.mult, [a, o], [m]),
                            trs,
                            lambda: self.cp(stg.t[:, :, jt * 128:(jt + 1) * 128], pt.t[:, 0:512].rearrange("p (b t) -> p b t", b=4), [pt], [stg], eng="act")]
                    return sts
                jobs.append(job)
            for g0 in range(0, len(jobs), 2):
                self.emit_interleaved([jb() for jb in jobs[g0:g0 + 2]], 2)
            S.dma("sp", mT[:, u0:u0 + n].rearrange("(b p) t -> p b t", p=128), stg.t[:, :, 0:n], reads=[stg])
        S.phase_end()

    def phase_merge(self, l):
        S, nc, cfg = self.S, self.nc, self.cfg
        T, TC, NT = cfg.T, cfg.TC, cfg.NT
        d = self.dscr
        last = (l == DEPTH - 1)
        moe = (l % 2 == 1)
        brT = [d("aT", [512, NT], BF16), d("bT", [512, NT], BF16), d("mT", [512, NT], BF16)]
        gT = d("gT", [3072, NT], BF16)
        xmid = d("xmid", [NT, DM], F32)
        xn2T = d("xn2T", [DM, NT], BF16)
        gates = d("gates", [NT, N_EXP], F32)
        S.phase_begin()
        wbr = []
        for i, nm in enumerate(("w_br_attn", "w_br_diff", "w_br_mlstm")):
            w = S.sb("wbr%d" % i, [128, 4, DM], BF16)
            S.dma("pool", w.t[:], self.W[nm][l].rearrange("(k p) n -> p k n", p=128), writes=[w])
            wbr.append(w)
        wo = S.sb("wo", [128, KT, DM], BF16)
        S.dma("pool", wo.t[:], self.W["w_out"][l].rearrange("(k p) n -> p k n", p=128), writes=[wo])
        mods = {}
        tmps = [S.sb("dtmp%d" % k, [128, DM], F32) for k in range(1)]
        gsh = tmps[0]
        for which in ((0,) if last else (1, 0)):
            G1 = S.sb("G1_%d" % which, [128, DM], F32)
            self.load_bc(G1, self.mod[l, which, 2 * DM:3 * DM]); self.load_bc(gsh, self.W["post_mix_g"][l])
            self.tt(G1.t[:], G1.t[:], gsh.t[:], ALU.mult, [G1, gsh], [G1])
            A2, SH2 = self.make_mod_tiles(l, "pre_ffn_g", 4, 3, which, "d%d" % which, gbuf=gsh)
            mods[which] = (G1, A2, SH2)
        if moe:
            wr = S.sb("wr", [128, KT, N_EXP], F32)
            S.dma("sp", wr.t[:], self.W["w_router"][0].rearrange("(k p) e -> p k e", p=128), writes=[wr])
            br_ = S.sb("brt", [128, N_EXP], F32)
            self.load_bc(br_, self.W["b_router"][0])
            xnfs = [S.sb("xnf%d" % k, [128, DM], F32) for k in range(2)]; xnfT = S.sb("xnfT", [128, KT, 128], F32)
            lgs = [S.sb("lg%d" % k, [128, 8], F32) for k in range(2)]; mxs = [S.sb("mx8_%d" % k, [128, 8], F32) for k in range(2)]
            wvs = [S.sb("wv%d" % k, [128, 4], F32) for k in range(2)]
            m1s = [S.sb("gm1_%d" % k, [128, 8], F32) for k in range(2)]; m2s = [S.sb("gm2_%d" % k, [128, 8], F32) for k in range(2)]
        brb = [[S.sb("brb%d_%d" % (i, k), [128, 4, 512], BF16) for k in range(2)] for i in range(3)]
        gb = [S.sb("gtb%d" % k, [128, 24, 512], BF16) for k in range(2)]
        zT = [S.sb("zT%d" % k, [128, KT, 512], BF16) for k in range(2)]
        t1 = [S.sb("dt1_%d" % k, [128, 512], F32) for k in range(2)]
        t2 = [S.sb("dt2_%d" % k, [128, 512], F32) for k in range(2)]
        xt = [S.sb("dxt%d" % k, [128, DM], F32) for k in range(2)]
        x1 = [S.sb("dx1%d" % k, [128, DM], F32) for k in range(2)]
        junks = [S.sb("djunk%d" % k, [128, 512], F32) for k in range(1)]
        st = [S.sb("dst%d" % k, [128, 4], F32) for k in range(2)]
        st2 = [S.sb("dst2%d" % k, [128, 2], F32) for k in range(2)]
        xb = [S.sb("dxb%d" % k, [128, DM], BF16) for k in range(2)]
        stT = [S.sb("dstT%d" % k, [128, KT, 512], BF16) for k in range(1)]
        pend = []
        for (u0, n, isc) in cfg.blocks:
            if isc and last:
                continue
            G1, A2, SH2 = mods[1 if isc else 0]
            bt = []
            for i in range(3):
                b = self.rr("brb%d" % i, brb[i])
                S.dma("sp", b.t[:, :, 0:n], brT[i][:, u0:u0 + n].rearrange("(k p) t -> p k t", p=128), writes=[b])
                bt.append(b)
            g = self.rr("gtb", gb)
            S.dma("sp", g.t[:, :, 0:n], gT[:, u0:u0 + n].rearrange("(k p) t -> p k t", p=128), writes=[g])
            z = self.rr("zT", zT)
            for ft in range(8):
                pss = []
                for i in range(3):
                    ps = self.rr("pb", self.pb)
                    for kt in range(4):
                        self.mm(ps.t[:, 0:n], wbr[i].t[:, kt, ft * 128:(ft + 1) * 128], bt[i].t[:, kt, 0:n], kt == 0, kt == 3,
                                [wbr[i], bt[i]], [ps])
                    pss.append(ps)
                a1 = self.rr("dt1", t1); a2 = self.rr("dt2", t2)
                self.tt(a1.t[:, 0:n], pss[0].t[:, 0:n], g.t[:, 0 + ft, 0:n], ALU.mult, [pss[0], g], [a1])
                self.tt(a2.t[:, 0:n], pss[1].t[:, 0:n], g.t[:, 8 + ft, 0:n], ALU.mult, [pss[1], g], [a2])
                self.tt(a1.t[:, 0:n], a1.t[:, 0:n], a2.t[:, 0:n], ALU.add, [a1, a2], [a1], eng="pool")
                self.tt(a2.t[:, 0:n], pss[2].t[:, 0:n], g.t[:, 16 + ft, 0:n], ALU.mult, [pss[2], g], [a2])
                self.tt(z.t[:, ft, 0:n], a1.t[:, 0:n], a2.t[:, 0:n], ALU.add, [a1, a2], [z], eng="pool")
            stg = self.rr("dstT", stT)
            for j in range(n // 128):
                u = u0 + j * 128
                x = self.rr("dxt", xt); xo = self.rr("dx1", x1); s_ = self.rr("dst", st); s2 = self.rr("dst2", st2); xb_ = self.rr("dxb", xb)
                S.dma("sp", x.t[:], self.x_src(l, u), writes=[x])
                pp = []
                tmp = self.rr("dtmp", tmps)
                for ch in range(2):
                    junk = self.rr("djunk", junks)
                    ps = self.rr("pb", self.pb)
                    for kt in range(KT):
                        self.mm(ps.t[:, :], z.t[:, kt, j * 128:(j + 1) * 128], wo.t[:, kt, ch * 512:(ch + 1) * 512], kt == 0, kt == KT - 1,
                                [z, wo], [ps])
                    self.act(junk.t[:], ps.t[:], AF.Square, [ps], [junk, s_], accum_out=s_.t[:, ch:ch + 1])
                    pp.append(ps)
                self.tt(s_.t[:, 2:3], s_.t[:, 0:1], s_.t[:, 1:2], ALU.add, [s_], [s_])
                self.rstd(s_.t[:, 3:4], s_.t[:, 2:3], DM, s_)
                for ch in range(2):
                    cs = slice(ch * 512, (ch + 1) * 512)
                    self.stt(tmp.t[:, cs], pp[ch].t[:], s_.t[:, 3:4], G1.t[:, cs], ALU.mult, ALU.mult, [pp[ch], s_, G1], [tmp])
                self.tt(xo.t[:], tmp.t[:], x.t[:], ALU.add, [tmp, x], [xo])
                S.dma("sp", xmid[u:u + 128, :], xo.t[:], reads=[xo])
                if moe:
                    xnf = self.rr("xnf", xnfs)
                    self.rms_mod(xo, A2, SH2, xnf, tmp, s2)
                    self.cp(xb_.t[:], xnf.t[:], [xnf], [xb_], eng="act")

                def router(xnf=(xnf if moe else None), u=u):
                    lg = self.rr("lg", lgs); mx = self.rr("mx8", mxs); wv = self.rr("wv", wvs); m1 = self.rr("gm1", m1s); m2 = self.rr("gm2", m2s)
                    for kt in range(KT):
                        ps = self.rr("pb", self.pb)
                        self.tr(ps.t[:, 0:128], xnf.t[:, kt * 128:(kt + 1) * 128], self.ident_f.t[:], [xnf, self.ident_f], [ps])
                        self.cp(xnfT.t[:, kt, :], ps.t[:, 0:128], [ps], [xnfT], eng=("act" if kt % 2 else "dve"))
                    ps = self.rr("pb", self.pb)
                    for kt in range(KT):
                        self.mm(ps.t[:, 0:N_EXP], xnfT.t[:, kt, :], wr.t[:, kt, :], kt == 0, kt == KT - 1, [xnfT, wr], [ps])
                    self.tt(lg.t[:], ps.t[:, 0:N_EXP], br_.t[:], ALU.add, [ps, br_], [lg])
                    S.op("dve", lambda: nc.vector.max(mx.t[:], lg.t[:]), [lg], [mx])
                    self.tt(wv.t[:, 0:1], mx.t[:, 1:2], mx.t[:, 0:1], ALU.subtract, [mx], [wv])
                    self.act(wv.t[:, 0:1], wv.t[:, 0:1], AF.Exp, [wv], [wv])
                    self.ts(wv.t[:, 0:1], wv.t[:, 0:1], 1.0, None, ALU.add, None, [wv], [wv])
                    self.recip(wv.t[:, 1:2], wv.t[:, 0:1], [wv], [wv])
                    self.ts(wv.t[:, 2:3], wv.t[:, 1:2], -1.0, 1.0, ALU.mult, ALU.add, [wv], [wv])
                    self.ts(m1.t[:], lg.t[:], mx.t[:, 0:1], wv.t[:, 1:2], ALU.is_equal, ALU.mult, [lg, mx, wv], [m1])
                    self.ts(m2.t[:], lg.t[:], mx.t[:, 1:2], wv.t[:, 2:3], ALU.is_equal, ALU.mult, [lg, mx, wv], [m2])
                    self.tt(m1.t[:], m1.t[:], m2.t[:], ALU.add, [m1, m2], [m1])
                    S.dma("sp", gates[u:u + 128, :], m1.t[:], reads=[m1])
                if not moe:
                    self.rms_mod(xo, A2, SH2, xb_, tmp, s2)

                def tail(xb_=xb_, stg=stg, j=j, u0=u0, n=n, router=router):
                    if moe:
                        router()
                    pt = self.rr("pt", self.pt)
                    for kt in range(KT):
                        self.tr(pt.t[:, kt * 128:(kt + 1) * 128], xb_.t[:, kt * 128:(kt + 1) * 128], self.ident_b.t[:], [xb_, self.ident_b], [pt],
                                inc=(kt == KT - 1))
                    self.cp(stg.t[:, :, j * 128:(j + 1) * 128], pt.t[:].rearrange("p (k t) -> p k t", k=KT), [pt], [stg], eng="act")
                    if j == n // 128 - 1:
                        S.dma("sp", xn2T[:, u0:u0 + n].rearrange("(k p) t -> p k t", p=128), stg.t[:, :, 0:n], reads=[stg])
                pend.append(tail)
                while len(pend) > 1:
                    pend.pop(0)()
        while pend:
            pend.pop(0)()
        S.phase_end()

    def phase_ffn(self, l):
        S, nc, cfg = self.S, self.nc, self.cfg
        T, TC, NT = cfg.T, cfg.TC, cfg.NT
        d = self.dscr
        last = (l == DEPTH - 1)
        moe = (l % 2 == 1)
        j_ = l // 2
        xmid = d("xmid", [NT, DM], F32); xn2T = d("xn2T", [DM, NT], BF16); gates = d("gates", [NT, N_EXP], F32)
        facc = d("facc", [NT, DM], F32)
        units = []
        if not moe:
            for (h0, nh) in ((0, 6), (6, 6), (12, 5), (17, 5)):
                units.append((self.W["w_ff_gate"][j_], self.W["w_ff_up"][j_], self.W["w_ff_down"][j_], h0, nh, None))
        else:
            for e in range(N_EXP):
                for (h0, nh) in ((0, 6), (6, 5)):
                    units.append((self.W["w_moe_gate"][j_, e], self.W["w_moe_up"][j_, e], self.W["w_moe_down"][j_, e], h0, nh, e))
        S.phase_begin()
        wgb = [S.sb("wg%d" % k, [128, KT, 768], BF16) for k in range(2)]
        wub = [S.sb("wu%d" % k, [128, KT, 768], BF16) for k in range(2)]
        wdb = [S.sb("wd%d" % k, [128, 6, DM], BF16) for k in range(2)]
        xb = [S.sb("exn%d" % k, [128, KT, 512], BF16) for k in range(2)]
        hT = [S.sb("ehT%d" % k, [128, 6, 512], BF16) for k in range(2)]
        sg = [S.sb("esg%d" % k, [128, 512], F32) for k in range(2)]
        ob = [S.sb("eo%d" % k, [128, DM], F32) for k in range(3)]
        gtl = [S.sb("egt%d" % k, [128, 4, N_EXP], F32) for k in range(2)]
        facc_b = {}
        blocks = [b for b in cfg.blocks if not (b[2] and last)]
        for ui, (wg_ap, wu_ap, wd_ap, h0, nh, ex) in enumerate(units):
            wg = self.rr("wg", wgb); wu = self.rr("wu", wub); wd = self.rr("wd", wdb)
            S.dma("pool", wg.t[:, :, 0:nh * 128], wg_ap.rearrange("(k p) n -> p k n", p=128)[:, :, h0 * 128:(h0 + nh) * 128], writes=[wg])
            S.dma("pool", wu.t[:, :, 0:nh * 128], wu_ap.rearrange("(k p) n -> p k n", p=128)[:, :, h0 * 128:(h0 + nh) * 128], writes=[wu])
            S.dma("pool", wd.t[:, 0:nh, :], wd_ap[h0 * 128:(h0 + nh) * 128, :].rearrange("(k p) n -> p k n", p=128), writes=[wd])
            for (u0, n, isc) in blocks:
                x = self.rr("exn", xb)
                S.dma("sp", x.t[:, :, 0:n], xn2T[:, u0:u0 + n].rearrange("(k p) t -> p k t", p=128), writes=[x])
                if ex is not None:
                    gt = self.rr("egt", gtl)
                    S.dma("sp", gt.t[:, 0:n // 128, :], gates[u0:u0 + n, :].rearrange("(j p) e -> p j e", p=128), writes=[gt])
                h = self.rr("ehT", hT)
                for ht in range(nh):
                    pg = self.rr("e_pg", [self.pb[0], self.pb[1]]); pu = self.rr("e_pu", [self.pb[2], self.pb[3]])
                    for kt in range(KT):
                        self.mm(pg.t[:, 0:n], wg.t[:, kt, ht * 128:(ht + 1) * 128], x.t[:, kt, 0:n], kt == 0, kt == KT - 1, [wg, x], [pg])
                    for kt in range(KT):
                        self.mm(pu.t[:, 0:n], wu.t[:, kt, ht * 128:(ht + 1) * 128], x.t[:, kt, 0:n], kt == 0, kt == KT - 1, [wu, x], [pu])
                    s_ = self.rr("esg", sg)
                    self.act(s_.t[:, 0:n], pg.t[:, 0:n], AF.Silu, [pg], [s_])
                    self.tt(h.t[:, ht, 0:n], s_.t[:, 0:n], pu.t[:, 0:n], ALU.mult, [s_, pu], [h])
                for j in range(n // 128):
                    u = u0 + j * 128
                    o = self.rr("eo", ob)
                    for ch in range(2):
                        ps = self.rr("e_pd", [self.pb[4], self.pb[5]])
                        for ht in range(nh):
                            self.mm(ps.t[:, :], h.t[:, ht, j * 128:(j + 1) * 128], wd.t[:, ht, ch * 512:(ch + 1) * 512], ht == 0, ht == nh - 1,
                                    [h, wd], [ps])
                        if ex is None:
                            self.cp(o.t[:, ch * 512:(ch + 1) * 512], ps.t[:], [ps], [o], eng=("act" if ch else "dve"))
                        else:
                            self.act(o.t[:, ch * 512:(ch + 1) * 512], ps.t[:], AF.Copy, [ps, gt], [o], scale=gt.t[:, j, ex:ex + 1])
                    fb = facc_b.setdefault(u, Buf("facc%d" % u))
                    if ui == 0:
                        S.dma("pool", facc[u:u + 128, :], o.t[:], reads=[o], writes=[fb])
                    else:
                        S.dma("pool", facc[u:u + 128, :], o.t[:], reads=[o], writes=[fb], accum_op=ALU.add)
        S.phase_end()
        S.phase_begin()
        G2 = {}
        for which in ((0,) if last else (1, 0)):
            G = S.sb("G2_%d" % which, [128, DM], F32); g = S.sb("gpf_%d" % which, [128, DM], F32)
            self.load_bc(G, self.mod[l, which, 5 * DM:6 * DM]); self.load_bc(g, self.W["post_ffn_g"][l])
            self.tt(G.t[:], G.t[:], g.t[:], ALU.mult, [G, g], [G])
            G2[which] = G
        fa = [S.sb("fa%d" % k, [128, DM], F32) for k in range(2)]
        xm = [S.sb("fxm%d" % k, [128, DM], F32) for k in range(2)]
        xo = [S.sb("fxo%d" % k, [128, DM], F32) for k in range(2)]
        ftmp = [S.sb("ftmp%d" % k, [128, DM], F32) for k in range(2)]
        st = [S.sb("fst%d" % k, [128, 2], F32) for k in range(2)]
        jobs = []
        for (u0, n, isc) in blocks:
            for j in range(n // 128):
                def job(u=u0 + j * 128, isc=isc):
                    f = self.rr("fa", fa); x = self.rr("fxm", xm); o = self.rr("fxo", xo); s_ = self.rr("fst", st); tmp = self.rr("ftmp", ftmp)
                    if last:
                        dst = self.out[u - TC:u - TC + 128, :]
                    elif isc:
                        dst = self.cres[u:u + 128, :]
                    else:
                        dst = self.xres[u - TC:u - TC + 128, :]

                    def loads():
                        S.dma("sp", f.t[:], facc[u:u + 128, :], writes=[f])
                        S.dma("sp", x.t[:], xmid[u:u + 128, :], writes=[x])
                    sts = [loads]
                    sts += self.rms_mod_stages(f, G2[1 if isc else 0], None, tmp, tmp, s_)
                    sts.append(lambda: self.tt(o.t[:], tmp.t[:], x.t[:], ALU.add, [tmp, x], [o]))
                    sts.append(lambda: S.dma("sp", dst, o.t[:], reads=[o]))
                    return sts
                jobs.append(job)
        for g0 in range(0, len(jobs), 2):
            self.emit_interleaved([jb() for jb in jobs[g0:g0 + 2]], 2)
        S.phase_end()


def rope_tables(T):
    GRID_W = 64
    t = np.arange(T)
    row = (t // GRID_W).astype(np.float32)
    col = (t % GRID_W).astype(np.float32)
    inv = (10000.0 ** (-np.arange(16, dtype=np.float32) / 16)).astype(np.float32)
    ar = row[:, None] * inv
    ac = col[:, None] * inv
    C = np.concatenate([np.cos(ar), np.cos(ar), np.cos(ac), np.cos(ac)], axis=1).astype(np.float32)
    Sn = np.concatenate([-np.sin(ar), np.sin(ar), -np.sin(ac), np.sin(ac)], axis=1).astype(np.float32)
    return C, Sn


def build_program(cfg):
    B = Builder(cfg)
    B.phase_mod()
    for l in range(DEPTH):
        B.phase_A(l)
        if cfg.stop_after == "A":
            break
        B.phase_attn(l, "a")
        B.phase_attn(l, "b")
        if cfg.stop_after == "B":
            break
        B.phase_mlstm(l)
        if cfg.stop_after == "C":
            break
        B.phase_merge(l)
        if cfg.stop_after == "D":
            break
        B.phase_ffn(l)
        if cfg.stop_after == "E":
            break
    B.S.finish()
    return B


def make_in_maps(inputs, cfg, n_cores):
    f = lambda a: np.ascontiguousarray(np.asarray(a, dtype=np.float32))
    C, Sn = rope_tables(cfg.T)
    shared = {k: f(v) for k, v in inputs.items() if k not in ("x", "c", "ctx", "c_ctx")}
    shared["ropeC"] = C
    shared["ropeS"] = Sn
    shared["ident"] = np.eye(128, dtype=np.float32)
    s_ = np.arange(64)[:, None]; t_ = np.arange(64)[None, :]
    shared["mlmask"] = np.ascontiguousarray(np.stack([(s_ <= t_), (s_ >= t_)], axis=1).astype(np.float32))
    shared["cctxT"] = f(np.asarray(inputs["c_ctx"]).reshape(KT, 128).T)
    maps = []
    for b in range(n_cores):
        m = dict(shared)
        m["x"] = f(inputs["x"][b])
        m["ctx"] = f(inputs["ctx"][b])
        m["cT"] = f(np.asarray(inputs["c"][b]).reshape(KT, 128).T)
        maps.append(m)
    return maps


def kernel(**inputs):
    cfg = Cfg()
    B = build_program(cfg)
    maps = make_in_maps(inputs, cfg, 8)
    res = run_bass_kernel_spmd(B.nc, maps, core_ids=list(range(8)))
    return np.stack([np.asarray(r["out"]) for r in res.results], axis=0).astype(np.float32)
```

```python
import numpy as np
import concourse.bass as bass
import concourse.mybir as mybir
from concourse.bass_utils import run_bass_kernel_spmd

F32 = mybir.dt.float32
BF16 = mybir.dt.bfloat16
AF = mybir.ActivationFunctionType
ALU = mybir.AluOpType
AX = mybir.AxisListType


class Buf:
    __slots__ = ("name", "t", "w", "r")

    def __init__(self, name, t=None):
        self.name = name
        self.t = t
        self.w = None
        self.r = []


EPOCH = 20000
NDMA = 24


class Sched:
    def __init__(self, nc):
        self.nc = nc
        self.eng = {"pe": nc.tensor, "act": nc.scalar, "dve": nc.vector, "pool": nc.gpsimd, "sp": nc.sync}
        self.cnt = {e: 0 for e in self.eng}
        self.pending = {e: [] for e in self.eng}
        self.sems = {e: [] for e in self.eng}
        self.seen = {e: {} for e in self.eng}
        self.dma_sems = {}
        self.dma_cnt = {}
        self.dma_last = {}
        self._stack = []
        self.nwaits = 0
        self.nops = 0

    def _sem(self, name):
        g = self.nc.semaphore(name)
        s = g.__enter__()
        self._stack.append(g)
        return s

    def sb(self, name, shape, dtype):
        self._uid = getattr(self, "_uid", 0) + 1
        g = self.nc.sbuf_tensor("%s_%d" % (name, self._uid), shape, dtype)
        t = g.__enter__()
        (self._phase if self._phase is not None else self._stack).append(g)
        return Buf(name, t)

    _phase = None

    def phase_begin(self):
        assert self._phase is None
        self._phase = []

    def phase_end(self):
        self.barrier()
        for g in reversed(self._phase):
            g.__exit__(None, None, None)
        self._phase = None

    def ps(self, name, shape, dtype=F32):
        g = self.nc.psum_tensor(name, shape, dtype)
        t = g.__enter__()
        self._stack.append(g)
        return Buf(name, t)

    def _eng_sem(self, e, idx):
        ep = (idx - 1) // EPOCH
        while len(self.sems[e]) <= ep:
            self.sems[e].append(self._sem("s_%s_%d" % (e, len(self.sems[e]))))
        return self.sems[e][ep], (idx - 1) % EPOCH + 1, ep

    def _wait(self, e, tok):
        if tok is None:
            return
        if tok[0] == "dma":
            _, q, slot, val = tok
            key = ("dma", q, slot)
            if self.seen[e].get(key, 0) >= val:
                return
            self.seen[e][key] = val
            self.eng[e].wait_ge(self.dma_sems[(q, slot)], val)
            self.nwaits += 1
            return
        _, e2, ref = tok
        if e2 == e and e == "pe":
            return
        idx = ref[0]
        if idx is None:
            raise RuntimeError("dependency on un-marked op of engine %s" % e2)
        key = ("eng", e2)
        if self.seen[e].get(key, 0) >= idx:
            return
        self.seen[e][key] = idx
        sem, val, ep = self._eng_sem(e2, idx)
        self.eng[e].wait_ge(sem, val)
        self.nwaits += 1

    def _deps(self, e, reads, writes):
        for b in reads:
            self._wait(e, b.w)
        for b in writes:
            self._wait(e, b.w)
            for r in b.r:
                self._wait(e, r)

    def _commit(self, tok, reads, writes):
        for b in reads:
            b.r.append(tok)
            if len(b.r) > 64:
                b.r = b.r[-64:]
        for b in writes:
            b.w = tok
            b.r = []

    def op(self, e, fn, reads=(), writes=(), inc=True):
        self._deps(e, reads, writes)
        ins = fn()
        self.nops += 1
        ref = [None]
        tok = ("eng", e, ref)
        self.pending[e].append(ref)
        if inc:
            self.cnt[e] += 1
            idx = self.cnt[e]
            sem, val, ep = self._eng_sem(e, idx)
            ins.then_inc(sem, 1)
            for r in self.pending[e]:
                r[0] = idx
            self.pending[e] = []
        self._commit(tok, reads, writes)
        return tok

    def dma(self, q, out, in_, reads=(), writes=(), **kw):
        n = self.dma_cnt.get(q, 0)
        self.dma_cnt[q] = n + 1
        slot = n % NDMA
        if (q, slot) not in self.dma_sems:
            self.dma_sems[(q, slot)] = self._sem("d_%s_%d" % (q, slot))
        val = 16 * (n // NDMA + 1)
        if val > 16:
            self._wait(q, ("dma", q, slot, val - 16))
        self._deps(q, reads, writes)
        ins = self.eng[q].dma_start(out=out, in_=in_, **kw)
        ins.then_inc(self.dma_sems[(q, slot)], 16)
        self.nops += 1
        tok = ("dma", q, slot, val)
        self._commit(tok, reads, writes)
        self.dma_last.setdefault(q, []).append(tok)
        return tok

    def barrier(self):
        toks = []
        for e in ("pe", "act", "dve", "pool"):
            if self.cnt[e] > 0:
                if self.pending[e]:
                    raise RuntimeError("barrier with un-marked pending ops on %s" % e)
                toks.append(("eng", e, [self.cnt[e]]))
        for q, lst in self.dma_last.items():
            toks.extend(lst[-NDMA:])
        for e in self.eng:
            for t in toks:
                if t[0] == "eng" and t[1] == e:
                    continue
                self._wait(e, t)

    def finish(self):
        self.barrier()
        for g in reversed(self._stack):
            g.__exit__(None, None, None)
        self._stack = []


DM = 1024
KT = 8
EPS = 1e-6
N_IN = 7440
C_AQ, C_AK, C_AV, C_BQ, C_BK, C_BV, C_CQ, C_CK, C_CV, C_CO, C_CG, C_GATE = (
    0, 512, 640, 768, 1280, 1792, 2304, 2816, 3328, 3840, 4352, 4368)
D_FF = 2816
N_EXP = 8
D_FFE = 1408
DEPTH = 2


class Cfg:
    def __init__(self, T=4096, TC=256, debug=False, stop_after=None):
        self.T, self.TC, self.NT = T, TC, T + TC
        self.debug = debug
        self.stop_after = stop_after
        self.blocks = []
        u = 0
        while u < TC:
            n = min(512, TC - u)
            self.blocks.append((u, n, True))
            u += n
        while u < self.NT:
            n = min(512, self.NT - u)
            self.blocks.append((u, n, False))
            u += n


class Builder:
    def __init__(self, cfg):
        self.cfg = cfg
        nc = self.nc = bass.Bass("TRN2", target_bir_lowering=False)
        S = self.S = Sched(nc)
        T, TC, NT = cfg.T, cfg.TC, cfg.NT
        self.inputs = {}
        self.outputs = {}

        def din(name, shape, dt=F32):
            ap = nc.dram_tensor(name, list(shape), dt, kind="ExternalInput").ap()
            self.inputs[name] = ap
            return ap
        self.din = din
        self.x_in = din("x", [T, DM])
        self.ctx_in = din("ctx", [TC, DM])
        self.cT = din("cT", [128, KT])
        self.cctxT = din("cctxT", [128, KT])
        self.ropeC = din("ropeC", [T, 64])
        self.ropeS = din("ropeS", [T, 64])
        W = self.W = {}
        for nm, shp in (("ada_w", [DEPTH, DM, 6 * DM]), ("ada_b", [DEPTH, 6 * DM]), ("pre_mix_g", [DEPTH, DM]),
                        ("post_mix_g", [DEPTH, DM]), ("pre_ffn_g", [DEPTH, DM]), ("post_ffn_g", [DEPTH, DM]),
                        ("w_in", [DEPTH, DM, N_IN]), ("q_norm_g", [DEPTH, 64]), ("k_norm_g", [DEPTH, 64]),
                        ("lam_q1", [DEPTH, 64]), ("lam_k1", [DEPTH, 64]), ("lam_q2", [DEPTH, 64]),
                        ("lam_k2", [DEPTH, 64]), ("diff_norm_g", [DEPTH, 128]), ("conv_w", [DEPTH, 3, 1024]),
                        ("conv_b", [DEPTH, 1024]), ("mlstm_gate_b", [DEPTH, 16]), ("mlstm_norm_g", [DEPTH, 128]),
                        ("w_br_attn", [DEPTH, 512, DM]), ("w_br_diff", [DEPTH, 512, DM]),
                        ("w_br_mlstm", [DEPTH, 512, DM]), ("w_out", [DEPTH, DM, DM]),
                        ("w_ff_gate", [1, DM, D_FF]), ("w_ff_up", [1, DM, D_FF]), ("w_ff_down", [1, D_FF, DM]),
                        ("w_router", [1, DM, N_EXP]), ("b_router", [1, N_EXP]),
                        ("w_moe_gate", [1, N_EXP, DM, D_FFE]), ("w_moe_up", [1, N_EXP, DM, D_FFE]),
                        ("w_moe_down", [1, N_EXP, D_FFE, DM])):
            W[nm] = din(nm, shp)
        self.out = nc.dram_tensor("out", [T, DM], F32, kind="ExternalOutput").ap()
        self.outputs["out"] = self.out
        self.scr = {}
        self.xres = self.dscr("xres", [T, DM], F32)
        self.cres = self.dscr("cres", [TC, DM], F32)
        self.PS = S.ps("PS", [128, 4096], F32)
        self.bank = [Buf("bank%d" % i, self.PS.t[:, i * 512:(i + 1) * 512]) for i in range(8)]
        self.pb = self.bank[0:6]
        self.pt = [Buf("pt%d" % i, self.bank[6 + i].t.bitcast(BF16)) for i in range(2)]
        self._rr = {}
        self.ident_b = S.sb("ident_b", [128, 128], BF16)
        self.ident_f = S.sb("ident_f", [128, 128], F32)
        self.ones_f = S.sb("ones_f", [128, 128], F32)
        self.ones_b = S.sb("ones_b", [128, 128], BF16)
        self.identd = din("ident", [128, 128])
        self.mlmask = din("mlmask", [64, 2, 64])
        S.dma("sp", self.ident_f.t[:], self.identd, writes=[self.ident_f])
        S.dma("pool", self.ident_b.t[:], self.identd, writes=[self.ident_b])
        S.op("dve", lambda: nc.vector.memset(self.ones_f.t[:], 1.0), writes=[self.ones_f])
        S.op("dve", lambda: nc.vector.memset(self.ones_b.t[:], 1.0), writes=[self.ones_b])

    def dscr(self, name, shape, dt):
        if name in self.scr:
            return self.scr[name]
        kind = "ExternalOutput" if self.cfg.debug else "Internal"
        ap = self.nc.dram_tensor(name, list(shape), dt, kind=kind).ap()
        self.scr[name] = ap
        if self.cfg.debug:
            self.outputs[name] = ap
        return ap

    def rr(self, key, lst):
        i = self._rr.get(key, 0)
        self._rr[key] = i + 1
        return lst[i % len(lst)]

    def mm(self, out, lhsT, rhs, start, stop, reads, writes):
        nc = self.nc
        return self.S.op("pe", lambda: nc.tensor.matmul(out, lhsT, rhs, start=start, stop=stop), reads, writes, inc=stop)

    def tr(self, out, in_, ident, reads, writes, inc=True):
        nc = self.nc
        return self.S.op("pe", lambda: nc.tensor.transpose(out, in_, ident), reads, writes, inc=inc)

    def act(self, out, in_, func, reads, writes, **kw):
        nc = self.nc
        return self.S.op("act", lambda: nc.scalar.activation(out, in_, func, **kw), reads, writes)

    def tt(self, out, a, b, op, reads, writes, eng="dve"):
        e = self.nc.vector if eng == "dve" else self.nc.gpsimd
        return self.S.op(eng, lambda: e.tensor_tensor(out, a, b, op), reads, writes)

    def ts(self, out, a, s1, s2, op0, op1, reads, writes, eng="dve"):
        e = self.nc.vector if eng == "dve" else self.nc.gpsimd
        if op1 is None:
            return self.S.op(eng, lambda: e.tensor_scalar(out, a, s1, None, op0), reads, writes)
        return self.S.op(eng, lambda: e.tensor_scalar(out, a, s1, s2, op0, op1), reads, writes)

    def stt(self, out, in0, scalar, in1, op0, op1, reads, writes):
        nc = self.nc
        return self.S.op("dve", lambda: nc.vector.scalar_tensor_tensor(out, in0, scalar, in1, op0, op1), reads, writes)

    def cp(self, out, in_, reads, writes, eng="dve"):
        if eng == "act":
            return self.act(out, in_, AF.Copy, reads, writes)
        e = self.nc.vector if eng == "dve" else self.nc.gpsimd
        return self.S.op(eng, lambda: e.tensor_copy(out, in_), reads, writes)

    def recip(self, out, in_, reads, writes):
        nc = self.nc
        return self.S.op("dve", lambda: nc.vector.reciprocal(out, in_), reads, writes)

    def recip_fast(self, out, in_, reads, writes):
        nc = self.nc
        return self.S.op("dve", lambda: nc.vector.reciprocal_approx_fast(out, in_), reads, writes)

    def rstd(self, out, ss, n, reads_writes):
        b = reads_writes
        self.ts(out, ss, 1.0 / n, EPS, ALU.mult, ALU.add, [b], [b])
        self.act(out, out, AF.Sqrt, [b], [b])
        self.recip(out, out, [b], [b])

    def phase_mod(self):
        S, nc = self.S, self.nc
        self.mod = self.dscr("mod", [DEPTH, 2, 6 * DM], F32)
        S.phase_begin()
        cs2 = S.sb("cs2", [128, KT, 2], F32)
        for which, src in ((0, self.cT), (1, self.cctxT)):
            b = S.sb("cs%d" % which, [128, KT], F32)
            S.dma("sp", b.t[:], src, writes=[b])
            self.act(cs2.t[:, :, which], b.t[:], AF.Silu, [b], [cs2])
        wb = [S.sb("adaw%d" % i, [128, KT, 512], F32) for i in range(3)]
        bT = S.sb("adabT", [128, 48], F32)
        bR = S.sb("adabR", [48, 128], F32)
        mT_ = S.sb("modT", [128, 2, 48], F32)
        mR = S.sb("modR", [96, 128], F32)
        NTL = 6 * DM // 128
        for l in range(DEPTH):
            wv = self.W["ada_w"][l].rearrange("(kt p) n -> p kt n", p=128)
            S.dma("sp", bR.t[:], self.W["ada_b"][l].rearrange("(t p) -> t p", p=128), writes=[bR])
            psb = self.rr("pb", self.pb)
            self.tr(psb.t[:, 0:NTL], bR.t[:], self.ident_f.t[0:NTL, 0:NTL], [bR, self.ident_f], [psb])
            self.cp(bT.t[:], psb.t[:, 0:NTL], [psb], [bT], eng="dve")
            ps = self.rr("pb", self.pb)
            for ch in range(12):
                w = self.rr("adaw", wb)
                S.dma("sp", w.t[:], wv[:, :, ch * 512:(ch + 1) * 512], writes=[w])
                for nt in range(4):
                    t = ch * 4 + nt
                    for kt in range(KT):
                        self.mm(ps.t[:, t:2 * NTL:NTL], w.t[:, kt, nt * 128:(nt + 1) * 128], cs2.t[:, kt, :], kt == 0, kt == KT - 1,
                                [w, cs2], [ps])
            self.tt(mT_.t[:], ps.t[:, 0:2 * NTL].rearrange("p (w t) -> p w t", w=2), bT.t[:].unsqueeze(1).broadcast_to([128, 2, NTL]),
                    ALU.add, [ps, bT], [mT_])
            ps2 = self.rr("pb", self.pb)
            self.tr(ps2.t[0:2 * NTL, 0:128], mT_.t[:].rearrange("p w t -> p (w t)"), self.ident_f.t[:], [mT_, self.ident_f], [ps2])
            self.cp(mR.t[:], ps2.t[0:2 * NTL, 0:128], [ps2], [mR], eng="act")
            for which in range(2):
                S.dma("sp", self.mod[l, which].rearrange("(t p) -> t p", p=128), mR.t[which * NTL:(which + 1) * NTL, :], reads=[mR])
        S.phase_end()

    def rstd_stages(self, out, ss, n, b):
        return [lambda: self.ts(out, ss, 1.0 / n, EPS, ALU.mult, ALU.add, [b], [b]),
                lambda: self.act(out, out, AF.Sqrt, [b], [b]),
                lambda: self.recip(out, out, [b], [b])]

    def rms_mod_stages(self, xt, A_bc, SH_bc, out, tmp, st):
        sts = [lambda: self.act(tmp.t[:], xt.t[:], AF.Square, [xt], [tmp, st], accum_out=st.t[:, 0:1])]
        sts += self.rstd_stages(st.t[:, 1:2], st.t[:, 0:1], DM, st)
        if SH_bc is None:
            sts.append(lambda: self.stt(out.t[:], xt.t[:], st.t[:, 1:2], A_bc.t[:], ALU.mult, ALU.mult, [xt, st, A_bc], [out]))
        else:
            sts.append(lambda: self.stt(tmp.t[:], xt.t[:], st.t[:, 1:2], A_bc.t[:], ALU.mult, ALU.mult, [xt, st, A_bc], [tmp]))
            sts.append(lambda: self.tt(out.t[:], tmp.t[:], SH_bc.t[:], ALU.add, [tmp, SH_bc], [out]))
        return sts

    @staticmethod
    def emit_interleaved(jobs, G):
        for g0 in range(0, len(jobs), G):
            grp = jobs[g0:g0 + G]
            for k in range(max(len(j) for j in grp)):
                for j in grp:
                    if k < len(j):
                        j[k]()

    def load_bc(self, buf, src_row):
        self.S.dma("sp", buf.t[:], src_row.partition_broadcast(128), writes=[buf])

    def rms_mod(self, xt, A_bc, SH_bc, out, tmp, st):
        self.act(tmp.t[:], xt.t[:], AF.Square, [xt], [tmp, st], accum_out=st.t[:, 0:1])
        self.rstd(st.t[:, 1:2], st.t[:, 0:1], DM, st)
        if SH_bc is None:
            self.stt(out.t[:], xt.t[:], st.t[:, 1:2], A_bc.t[:], ALU.mult, ALU.mult, [xt, st, A_bc], [out])
        else:
            self.stt(tmp.t[:], xt.t[:], st.t[:, 1:2], A_bc.t[:], ALU.mult, ALU.mult, [xt, st, A_bc], [tmp])
            self.tt(out.t[:], tmp.t[:], SH_bc.t[:], ALU.add, [tmp, SH_bc], [out])

    def make_mod_tiles(self, l, gname, sc_idx, sh_idx, which, tag, gbuf=None):
        S = self.S
        A = S.sb("A_" + tag, [128, DM], F32)
        SH = S.sb("SH_" + tag, [128, DM], F32)
        g = gbuf if gbuf is not None else S.sb("g_" + tag, [128, DM], F32)
        self.load_bc(A, self.mod[l, which, sc_idx * DM:(sc_idx + 1) * DM])
        self.load_bc(SH, self.mod[l, which, sh_idx * DM:(sh_idx + 1) * DM])
        self.load_bc(g, self.W[gname][l])
        self.stt(A.t[:], A.t[:], 1.0, g.t[:], ALU.add, ALU.mult, [A, g], [A])
        return A, SH

    def x_src(self, l, u0):
        TC = self.cfg.TC
        if u0 < TC:
            return (self.ctx_in if l == 0 else self.cres)[u0:u0 + 128, :]
        t0 = u0 - TC
        return (self.x_in if l == 0 else self.xres)[t0:t0 + 128, :]

    def phase_A(self, l):
        S, nc, cfg = self.S, self.nc, self.cfg
        T, TC, NT = cfg.T, cfg.TC, cfg.NT
        d = self.dscr
        aqT = d("aqT", [512, NT], BF16); akT = d("akT", [128, NT], BF16); av = d("av", [NT, 128], BF16)
        bqT = d("bqT", [512, NT], BF16); bkT = d("bkT", [512, NT], BF16); bv = d("bv", [NT, 512], BF16)
        cqkT = d("cqkT", [1024, NT], F32); cv = d("cv", [NT, 512], BF16); cog = d("cog", [NT, 512], BF16)
        cg = d("cg", [16, NT], F32); gT = d("gT", [3072, NT], BF16)
        S.phase_begin()
        xnT = S.sb("xnT", [128, KT, NT], BF16)
        xnT_blk = {}
        for (u0, n, isc) in cfg.blocks:
            xnT_blk[u0] = Buf("xnT_%d" % u0)
        mods = {}
        gsh = S.sb("gsh", [128, DM], F32)
        for which in (1, 0):
            mods[which] = self.make_mod_tiles(l, "pre_mix_g", 1, 0, which, "a%d" % which, gbuf=gsh)
        xt = [S.sb("xt%d" % i, [128, DM], F32) for i in range(2)]
        tmpn = [S.sb("tmpn%d" % i, [128, DM], F32) for i in range(2)]
        xnb = [S.sb("xnb%d" % i, [128, DM], BF16) for i in range(2)]
        st = [S.sb("st%d" % i, [128, 2], F32) for i in range(2)]
        jobs = []
        for (u0, n, isc) in cfg.blocks:
            for j in range(n // 128):
                def job(u0=u0, u=u0 + j * 128, isc=isc):
                    x = self.rr("xt", xt); xb = self.rr("xnb", xnb); s_ = self.rr("st", st); tmp = self.rr("tmpn", tmpn)
                    pt = self.rr("pt", self.pt)
                    A, SH = mods[1 if isc else 0]
                    sts = [lambda: S.dma("sp", x.t[:], self.x_src(l, u), writes=[x])]
                    sts += self.rms_mod_stages(x, A, SH, xb, tmp, s_)

                    def trs():
                        for kt in range(KT):
                            self.tr(pt.t[:, kt * 128:(kt + 1) * 128], xb.t[:, kt * 128:(kt + 1) * 128], self.ident_b.t[:],
                                    [xb, self.ident_b], [pt], inc=(kt == KT - 1))
                    sts.append(trs)
                    sts.append(lambda: self.cp(xnT.t[:, :, u:u + 128], pt.t[:].rearrange("p (k t) -> p k t", k=KT), [pt], [xnT_blk[u0]],
                                               eng=("act" if (u // 128) % 2 else "dve")))
                    return sts
                jobs.append(job)
        for g0 in range(0, len(jobs), 2):
            self.emit_interleaved([jb() for jb in jobs[g0:g0 + 2]], 2)
        if cfg.debug:
            dbg = d("dbg_xnT", [128, KT, NT], BF16)
            S.dma("sp", dbg, xnT.t[:], reads=list(xnT_blk.values()))
        wv = self.W["w_in"][l].rearrange("(kt p) n -> p kt n", p=128)
        wbuf = [S.sb("wch%d" % i, [128, KT, 512], BF16) for i in range(2)]
        latent_tiles = T // 128
        ropeC = S.sb("ropeC", [128, latent_tiles, 64], F32)
        ropeS = S.sb("ropeS", [128, latent_tiles, 64], F32)
        S.dma("sp", ropeC.t[:], self.ropeC.rearrange("(n p) d -> p n d", p=128), writes=[ropeC])
        S.dma("sp", ropeS.t[:], self.ropeS.rearrange("(n p) d -> p n d", p=128), writes=[ropeS])
        gq = S.sb("gq", [128, 64], F32); gk = S.sb("gk", [128, 64], F32)
        self.load_bc(gq, self.W["q_norm_g"][l]); self.load_bc(gk, self.W["k_norm_g"][l])
        f1 = [S.sb("f1_%d" % i, [128, 512], F32) for i in range(3)]
        f2 = [S.sb("f2_%d" % i, [128, 512], F32) for i in range(3)]
        f3 = [S.sb("f3_%d" % i, [128, 512], F32) for i in range(3)]
        ssb = [S.sb("ssb%d" % i, [128, 8], F32) for i in range(3)]
        ob = [S.sb("ob%d" % i, [128, 512], BF16) for i in range(4)]
        of = [S.sb("of%d" % i, [128, 512], F32) for i in range(2)]
        stT = [S.sb("stT%d" % i, [128, 4, 512], BF16) for i in range(2)]

        def load_w(c0, ncol):
            w = self.rr("wch", wbuf)
            S.dma("pool", w.t[:, :, 0:ncol], wv[:, :, c0:c0 + ncol], writes=[w])
            return w

        def rope(src, dst, H, lt):
            t1 = self.rr("f2", f2); t2 = self.rr("f3", f3)
            n = H * 64
            sv = src.t[:, 0:n].rearrange("p (h b j r) -> p h b j r", h=H, b=2, j=2)
            Cb = ropeC.t[:, lt, :].unsqueeze(1).broadcast_to([128, H, 64])
            self.tt(t1.t[:, 0:n].rearrange("p (h d) -> p h d", h=H), src.t[:, 0:n].rearrange("p (h d) -> p h d", h=H),
                    Cb, ALU.mult, [src, ropeC], [t1])
            t2v = t2.t[:, 0:n].rearrange("p (h b j r) -> p h b j r", h=H, b=2, j=2)
            Sv = ropeS.t[:, lt, :].rearrange("p (b j r) -> p b j r", b=2, j=2)
            for b in range(2):
                for j in range(2):
                    self.tt(t2v[:, :, b, j, :], sv[:, :, b, 1 - j, :],
                            Sv[:, b, j, :].unsqueeze(1).broadcast_to([128, H, 16]), ALU.mult, [src, ropeS], [t2])
            self.tt(dst.t[:, 0:n], t1.t[:, 0:n], t2.t[:, 0:n], ALU.add, [t1, t2], [dst])

        def headnorm(ps, H, g_bc, dst):
            n = H * 64
            sq = self.rr("f2", f2); ss = self.rr("ssb", ssb)
            self.act(sq.t[:, 0:n], ps.t[:, 0:n], AF.Square, [ps], [sq])
            self.S.op("dve", lambda: nc.vector.tensor_reduce(ss.t[:, 0:H], sq.t[:, 0:n].rearrange("p (h d) -> p h d", h=H),
                                                           AX.X, ALU.add), [sq], [ss])
            self.rstd(ss.t[:, 0:H], ss.t[:, 0:H], 64, ss)
            self.tt(sq.t[:, 0:n].rearrange("p (h d) -> p h d", h=H), ps.t[:, 0:n].rearrange("p (h d) -> p h d", h=H),
                    ss.t[:, 0:H].unsqueeze(2).broadcast_to([128, H, 64]), ALU.mult, [ps, ss], [sq])
            self.tt(dst.t[:, 0:n].rearrange("p (h d) -> p h d", h=H), sq.t[:, 0:n].rearrange("p (h d) -> p h d", h=H),
                    g_bc.t[:].unsqueeze(1).broadcast_to([128, H, 64]), ALU.mult, [sq, g_bc], [dst])

        def proj_tok(c0, ncol, epi):
            w = load_w(c0, ncol)
            pend = []
            for (u0, n, isc) in cfg.blocks:
                for j in range(n // 128):
                    u = u0 + j * 128
                    ps = self.rr("pb", self.pb)
                    for kt in range(KT):
                        self.mm(ps.t[:, 0:ncol], xnT.t[:, kt, u:u + 128], w.t[:, kt, 0:ncol], kt == 0, kt == KT - 1,
                                [xnT_blk[u0], w], [ps])
                    while len(pend) > 1:
                        pend.pop(0)()
                    tail = epi(ps, u, isc, j, (u0, n))
                    if tail is not None:
                        pend.append(tail)
            while pend:
                pend.pop(0)()

        def epi_T(dst_dram, row0, H, norm_g, do_rope):
            ncol = H * 64
            state = {}

            def epi(ps, u, isc, j, blk):
                u0, n = blk
                if norm_g is not None:
                    src = self.rr("f1", f1)
                    headnorm(ps, H, norm_g, src)
                else:
                    src = ps
                o = self.rr("ob", ob)
                if do_rope and not isc:
                    if src is ps:
                        src = self.rr("f1", f1)
                        self.cp(src.t[:, 0:ncol], ps.t[:, 0:ncol], [ps], [src], eng="act")
                    rope(src, o, H, (u - TC) // 128)
                else:
                    self.cp(o.t[:, 0:ncol], src.t[:, 0:ncol], [src], [o], eng="act")
                def tail():
                    if j == 0:
                        state["st"] = self.rr("stT", stT)
                    stg = state["st"]
                    nb = ncol // 128
                    pt = self.rr("pt", self.pt)
                    for b in range(nb):
                        self.tr(pt.t[:, b * 128:(b + 1) * 128], o.t[:, b * 128:(b + 1) * 128], self.ident_b.t[:],
                                [o, self.ident_b], [pt], inc=(b == nb - 1))
                    self.cp(stg.t[:, 0:nb, j * 128:(j + 1) * 128], pt.t[:, 0:nb * 128].rearrange("p (b t) -> p b t", b=nb),
                            [pt], [stg], eng="dve")
                    if j == n // 128 - 1:
                        S.dma("sp", dst_dram[row0:row0 + ncol, u0:u0 + n].rearrange("(b p) t -> p b t", p=128),
                              stg.t[:, 0:nb, 0:n], reads=[stg])
                return tail
            return epi

        def epi_copy(dst_dram, col0, ncol, func=None):
            def epi(ps, u, isc, j, blk):
                o = self.rr("ob", ob)
                if func is None:
                    self.cp(o.t[:, 0:ncol], ps.t[:, 0:ncol], [ps], [o], eng="act")
                else:
                    self.act(o.t[:, 0:ncol], ps.t[:, 0:ncol], func, [ps], [o])
                S.dma("sp", dst_dram[u:u + 128, col0:col0 + ncol], o.t[:, 0:ncol], reads=[o])
            return epi

        def proj_feat(c0, ncol, dst_dram, row0, func, dt):
            w = load_w(c0, ncol)
            for m0 in range(0, ncol, 128):
                m = min(128, ncol - m0)
                for (u0, n, isc) in cfg.blocks:
                    ps = self.rr("pb", self.pb)
                    for kt in range(KT):
                        self.mm(ps.t[0:m, 0:n], w.t[:, kt, m0:m0 + m], xnT.t[:, kt, u0:u0 + n], kt == 0, kt == KT - 1,
                                [xnT_blk[u0], w], [ps])
                    o = self.rr("ob", ob) if dt == BF16 else self.rr("of", of)
                    self.act(o.t[0:m, 0:n], ps.t[0:m, 0:n], func, [ps], [o])
                    S.dma("sp", dst_dram[row0 + m0:row0 + m0 + m, u0:u0 + n], o.t[0:m, 0:n], reads=[o])

        proj_tok(C_AQ, 512, epi_T(aqT, 0, 8, gq, True))
        proj_tok(C_AK, 128, epi_T(akT, 0, 2, gk, True))
        proj_tok(C_AV, 128, epi_copy(av, 0, 128))
        proj_tok(C_BQ, 512, epi_T(bqT, 0, 8, None, True))
        proj_tok(C_BK, 512, epi_T(bkT, 0, 8, None, True))
        proj_tok(C_BV, 512, epi_copy(bv, 0, 512))
        proj_tok(C_CV, 512, epi_copy(cv, 0, 512))
        proj_tok(C_CO, 512, epi_copy(cog, 0, 512, AF.Sigmoid))
        proj_feat(C_CQ, 512, cqkT, 0, AF.Copy, F32)
        proj_feat(C_CK, 512, cqkT, 512, AF.Copy, F32)
        proj_feat(C_CG, 16, cg, 0, AF.Copy, F32)
        for i in range(6):
            proj_feat(C_GATE + i * 512, 512, gT, i * 512, AF.Sigmoid, BF16)
        S.phase_end()


    def phase_attn(self, l, kind):
        S, nc, cfg = self.S, self.nc, self.cfg
        T, TC, NT = cfg.T, cfg.TC, cfg.NT
        ntile = NT // 128
        d = self.dscr
        last = (l == DEPTH - 1)
        lam_init = 0.8 - 0.6 * float(np.exp(-0.3 * l))
        if kind == "a":
            qT, kT, v, oT = d("aqT", [512, NT], BF16), d("akT", [128, NT], BF16), d("av", [NT, 128], BF16), d("aT", [512, NT], BF16)
            groups = [[h] for h in range(8)]
        else:
            qT, kT, v, oT = d("bqT", [512, NT], BF16), d("bkT", [512, NT], BF16), d("bv", [NT, 512], BF16), d("bT", [512, NT], BF16)
            groups = [[2 * h, 2 * h + 1] for h in range(4)]
        S.phase_begin()
        if kind == "a":
            V = S.sb("Vx", [128, ntile, 2, 128], BF16)
            for kh in range(2):
                S.dma("sp", V.t[:, :, kh, 0:64], v[:, kh * 64:(kh + 1) * 64].rearrange("(n p) c -> p n c", p=128), writes=[V])
            S.op("dve", lambda: nc.vector.memset(V.t[:, :, :, 64:128], 1.0), writes=[V])
        else:
            V = S.sb("V", [128, ntile, 512], BF16)
            S.dma("sp", V.t[:], v.rearrange("(n p) c -> p n c", p=128), writes=[V])
        qb = [S.sb("qTh%d" % i, [128, NT], BF16) for i in range(4)]
        kb = [S.sb("kTh%d" % i, [128, NT], BF16) for i in range(4)]
        pT = [S.sb("pT%d" % i, [128, 2, 512], BF16) for i in range(4)]
        fin = [S.sb("fin%d" % i, [128, 512], F32) for i in range(2)]
        rec = [S.sb("rec%d" % i, [128, 512], F32) for i in range(2)]
        osb = [S.sb("osb%d" % i, [128, 512], F32) for i in range(4)]
        ost = [S.sb("ost%d" % i, [128, 512], BF16) for i in range(2)]
        nsp = 3 if kind == "a" else 2
        spair = [(self.PS.t[:, i * 1024:(i + 1) * 1024], self.bank[2 * i], self.bank[2 * i + 1]) for i in range(nsp)]
        if kind == "a":
            obank = [self.bank[6], self.bank[7]]
        else:
            obank = [self.bank[4], self.bank[5]]
            dbank = [self.bank[6], self.bank[7]]
            accs = [S.sb("dacc%d" % i, [128, 2, 512], F32) for i in range(2)]
            PE_EVERY = 3
        if kind == "b":
            lv = {}
            for nm in ("lam_q1", "lam_k1", "lam_q2", "lam_k2"):
                lv[nm] = S.sb(nm, [128, 64], F32)
                self.load_bc(lv[nm], self.W[nm][l])
            lt = S.sb("lamtmp", [128, 64], F32)
            ls = S.sb("lams", [128, 4], F32)
            for i, (a, b) in enumerate((("lam_q1", "lam_k1"), ("lam_q2", "lam_k2"))):
                self.tt(lt.t[:], lv[a].t[:], lv[b].t[:], ALU.mult, [lv[a], lv[b]], [lt])
                S.op("dve", lambda: nc.vector.tensor_reduce(ls.t[:, i:i + 1], lt.t[:], AX.X, ALU.add), [lt], [ls])
            self.act(ls.t[:, 0:2], ls.t[:, 0:2], AF.Exp, [ls], [ls])
            self.tt(ls.t[:, 2:3], ls.t[:, 1:2], ls.t[:, 0:1], ALU.subtract, [ls], [ls])
            self.ts(ls.t[:, 2:3], ls.t[:, 2:3], -lam_init, None, ALU.add, None, [ls], [ls])
            gsub = S.sb("gsub", [128, 1], F32)
            S.dma("sp", gsub.t[:], self.W["diff_norm_g"][l].rearrange("(p o) -> p o", o=1), writes=[gsub])
            self.ts(gsub.t[:], gsub.t[:], 1.0 - lam_init, None, ALU.mult, None, [gsub], [gsub])
            sqbs = [S.sb("sqb%d" % i, [128, 512], F32) for i in range(2)]
            difs = [S.sb("dif%d" % i, [128, 512], F32) for i in range(2)]
        scale = 64 ** -0.5
        LA = 2 if kind == "a" else 1
        items = []
        cur_k = {}
        for grp in groups:
            for (u0, n, isc) in cfg.blocks:
                if isc and last:
                    continue
                ktiles = list(range(TC // 128)) if isc else list(range(ntile))
                pairs = [ktiles[i:i + 2] for i in range(0, len(ktiles), 2)]
                for gi, hq in enumerate(grp):
                    items.append(dict(grp=grp, gi=gi, hq=hq, u0=u0, n=n, pairs=pairs, first_of_grp=(u0 == [b for b in cfg.blocks if not (b[2] and last)][0][0])))
        loaded = {}

        def get_qk(it):
            key = tuple(it["grp"])
            if key not in loaded:
                qs, ks = [], []
                for hq in it["grp"]:
                    q = self.rr("qTh", qb)
                    for hf_ in range(2):
                        S.dma("sp", q.t[hf_ * 64:(hf_ + 1) * 64, :], qT[hq * 64:(hq + 1) * 64, :], writes=[q])
                    qs.append(q)
                    krow = (hq // 4) if kind == "a" else hq
                    if kind == "a" and cur_k.get("row") == krow:
                        ks.append(cur_k["buf"])
                    else:
                        k = self.rr("kTh", kb)
                        for hf_ in range(2):
                            S.dma("sp", k.t[hf_ * 64:(hf_ + 1) * 64, :], kT[krow * 64:(krow + 1) * 64, :], writes=[k])
                        cur_k["row"] = krow; cur_k["buf"] = k
                        ks.append(k)
                loaded[key] = (qs, ks)
            qs, ks = loaded[key]
            return qs[it["gi"]], ks[it["gi"]]
        grp_first = {}
        for it in items:
            grp_first.setdefault(tuple(it["grp"]), it)
        grp_order = list(grp_first.keys())

        units = [(it, pi) for it in items for pi in range(len(it["pairs"]))]

        def smm(unit):
            it, pi = unit
            if "q" not in it:
                it["q"], it["k"] = get_qk(it)
                gi_ = grp_order.index(tuple(it["grp"]))
                if it is grp_first[tuple(it["grp"])] and gi_ + 1 < len(grp_order):
                    get_qk(grp_first[grp_order[gi_ + 1]])
            q, k, n, u0 = it["q"], it["k"], it["n"], it["u0"]
            sp = self.rr("spair", spair)
            for hi, kt in enumerate(it["pairs"][pi]):
                rs = slice(hi * 64, (hi + 1) * 64)
                self.mm(sp[1 + hi].t[:, 0:n], k.t[rs, kt * 128:(kt + 1) * 128], q.t[rs, u0:u0 + n], True, True, [k, q], [sp[1 + hi]])
            return sp

        outs = []

        deferred = []
        r2s = [S.sb("r2_%d" % i, [64, 512], F32) for i in range(2)] if kind == "a" else None

        def finalize(it, ui):
            n, u0, hq, ob = it["n"], it["u0"], it["hq"], it["ob"]
            r = self.rr("rec", rec)
            if kind == "a":
                f = self.rr("fin", fin)
                self.cp(f.t[:, 0:n], ob.t[:, 0:n], [ob], [f], eng="dve")
                self.recip(r.t[64:128, 0:n], f.t[64:128, 0:n], [f], [r])
                r2 = self.rr("r2", r2s)
                S.dma("sp", r2.t[0:64, 0:n], r.t[64:128, 0:n], reads=[r], writes=[r2])

                def st2():
                    o = self.rr("ost", ost)
                    self.tt(o.t[0:64, 0:n], f.t[0:64, 0:n], r2.t[0:64, 0:n], ALU.mult, [f, r2], [o])
                    S.dma("sp", oT[hq * 64:(hq + 1) * 64, u0:u0 + n], o.t[0:64, 0:n], reads=[o])
                deferred.append((ui + 4, st2))
                return
            db, acc = it["db"], it["acc"]
            terms = [(acc, 0), (acc, 1)] if it["usedD"] else []
            for ti, (a_, hh) in enumerate(terms):
                self.mm(db.t[:, 0:n], self.ones_f.t[:], a_.t[:, hh, 0:n], (not it["den_started"]) and ti == 0, ti == len(terms) - 1,
                        [self.ones_f, a_], [db])
            assert terms, "every item accumulates at least one pair on DVE"
            self.act(r.t[:, 0:n], db.t[:, 0:n], AF.Ln, [db], [r])
            self.act(r.t[:, 0:n], r.t[:, 0:n], AF.Exp, [r], [r], scale=-1.0)
            o = self.rr("osb", osb)
            self.tt(o.t[:, 0:n], ob.t[:, 0:n], r.t[:, 0:n], ALU.mult, [ob, r], [o])
            ob1 = db
            outs.append(o)
            if len(outs) == 2:
                h = hq // 2
                o1, o2 = outs
                del outs[:]
                dif = self.rr("dif", difs); sqb = self.rr("sqb", sqbs)
                self.stt(dif.t[:, 0:n], o2.t[:, 0:n], ls.t[:, 2:3], o1.t[:, 0:n], ALU.mult, ALU.add, [o1, o2, ls], [dif])
                self.tt(sqb.t[:, 0:n], dif.t[:, 0:n], dif.t[:, 0:n], ALU.mult, [dif], [sqb])
                sb_ = ob1

                def st2():
                    self.mm(sb_.t[:, 0:n], self.ones_f.t[:], sqb.t[:, 0:n], True, True, [self.ones_f, sqb], [sb_])
                    self.ts(sqb.t[:, 0:n], sb_.t[:, 0:n], 1.0 / 128, EPS, ALU.mult, ALU.add, [sb_], [sqb])

                def st3():
                    self.act(sqb.t[:, 0:n], sqb.t[:, 0:n], AF.Ln, [sqb], [sqb])
                    self.act(sqb.t[:, 0:n], sqb.t[:, 0:n], AF.Exp, [sqb], [sqb], scale=-0.5)
                    oo = self.rr("ost", ost)
                    self.stt(oo.t[:, 0:n], dif.t[:, 0:n], gsub.t[:, 0:1], sqb.t[:, 0:n], ALU.mult, ALU.mult, [dif, gsub, sqb], [oo])
                    S.dma("sp", oT[h * 128:(h + 1) * 128, u0:u0 + n], oo.t[:, 0:n], reads=[oo])
                deferred.append((ui + 5, st2))
                deferred.append((ui + 9, st3))

        def run_deferred(ui):
            while deferred and deferred[0][0] <= ui:
                deferred.pop(0)[1]()

        queue = [smm(u) for u in units[0:LA]]
        for ui, (it, pi) in enumerate(units):
            sp = queue.pop(0)
            if ui + LA < len(units):
                queue.append(smm(units[ui + LA]))
            n, hq, pair = it["n"], it["hq"], it["pairs"][pi]
            npairs = len(it["pairs"])
            if pi == 0:
                it["ob"] = self.rr("obank", obank)
            ob = it["ob"]
            p = self.rr("pT", pT)
            np_ = len(pair)
            spv = sp[0].rearrange("p (h w) -> p h w", h=2)
            self.act(p.t[:, 0:np_, 0:n], spv[:, 0:np_, 0:n], AF.Exp, [sp[1], sp[2]][0:np_], [p], scale=scale)
            for hi, kt in enumerate(pair):
                first = (pi == 0 and hi == 0)
                lastk = (pi == npairs - 1 and hi == np_ - 1)
                if kind == "a":
                    self.mm(ob.t[:, 0:n], V.t[:, kt, hq // 4, :], p.t[:, hi, 0:n], first, lastk, [V, p], [ob])
                else:
                    self.mm(ob.t[:, 0:n], V.t[:, kt, (hq // 2) * 128:(hq // 2 + 1) * 128], p.t[:, hi, 0:n], first, lastk, [V, p], [ob])
            if kind == "b":
                if pi == 0:
                    it["db"] = self.rr("dbank", dbank); it["acc"] = self.rr("dacc", accs)
                    it["usedD"] = False; it["den_started"] = False
                db, acc = it["db"], it["acc"]
                if pi % PE_EVERY == PE_EVERY - 1 and pi != npairs - 1:
                    for hi in range(np_):
                        self.mm(db.t[:, 0:n], self.ones_b.t[:], p.t[:, hi, 0:n], not it["den_started"], False, [self.ones_b, p], [db])
                        it["den_started"] = True
                else:
                    if not it["usedD"]:
                        if np_ < 2:
                            S.op("dve", lambda: nc.vector.memset(acc.t[:, 1, 0:n], 0.0), writes=[acc])
                        self.cp(acc.t[:, 0:np_, 0:n], p.t[:, 0:np_, 0:n], [p], [acc], eng="dve")
                    else:
                        self.tt(acc.t[:, 0:np_, 0:n], acc.t[:, 0:np_, 0:n], p.t[:, 0:np_, 0:n], ALU.add, [p, acc], [acc])
                    it["usedD"] = True
            if pi == npairs - 1:
                finalize(it, ui)
            deferred.sort(key=lambda t: t[0])
            run_deferred(ui)
        run_deferred(10 ** 9)
        S.phase_end()

    def phase_mlstm(self, l):
        S, nc, cfg = self.S, self.nc, self.cfg
        T, TC, NT = cfg.T, cfg.TC, cfg.NT
        ntile = NT // 128
        nch = NT // 64
        cch = TC // 64
        d = self.dscr
        last = (l == DEPTH - 1)
        cqkT = d("cqkT", [1024, NT], F32); cv = d("cv", [NT, 512], BF16); cog = d("cog", [NT, 512], BF16)
        cg = d("cg", [16, NT], F32)
        cqT = d("cqT", [1024, NT], BF16); ktok = d("ktok", [NT, 512], BF16)
        hf = d("hf", [NT, 512], F32); hb = d("hb", [NT, 512], F32); mT = d("mT", [512, NT], BF16)
        S.phase_begin()
        xr = [S.sb("cx%d" % i, [128, NT], F32) for i in range(2)]
        yr = S.sb("cy", [128, NT], F32)
        yb = [S.sb("cyb%d" % i, [128, NT], BF16) for i in range(2)]
        cw = [S.sb("cw%d" % i, [128, 4], F32) for i in range(2)]
        kst = [S.sb("kst%d" % i, [128, 128], BF16) for i in range(3)]
        for j in range(8):
            x = self.rr("cx", xr); w = self.rr("cw", cw); y = self.rr("cyb", yb)
            S.dma("sp", x.t[:], cqkT[j * 128:(j + 1) * 128, :], writes=[x])
            for k in range(3):
                S.dma("sp", w.t[:, k:k + 1], self.W["conv_w"][l, k, j * 128:(j + 1) * 128].rearrange("(p o) -> p o", o=1), writes=[w])
            S.dma("sp", w.t[:, 3:4], self.W["conv_b"][l, j * 128:(j + 1) * 128].rearrange("(p o) -> p o", o=1), writes=[w])
            self.ts(yr.t[:], x.t[:], w.t[:, 1:2], w.t[:, 3:4], ALU.mult, ALU.add, [x, w], [yr])
            for (s, e) in ((0, TC), (TC, NT)):
                self.stt(yr.t[:, s + 1:e], x.t[:, s:e - 1], w.t[:, 0:1], yr.t[:, s + 1:e], ALU.mult, ALU.add, [x, w, yr], [yr])
                self.stt(yr.t[:, s:e - 1], x.t[:, s + 1:e], w.t[:, 2:3], yr.t[:, s:e - 1], ALU.mult, ALU.add, [x, w, yr], [yr])
            self.act(y.t[:], yr.t[:], AF.Silu, [yr], [y])
            S.dma("sp", cqT[j * 128:(j + 1) * 128, :], y.t[:], reads=[y])
            if j >= 4:
                for i in range(ntile):
                    pt = self.rr("pt", self.pt)
                    self.tr(pt.t[:, 0:128], y.t[:, i * 128:(i + 1) * 128], self.ident_b.t[:], [y, self.ident_b], [pt])
                    ks = self.rr("kst", kst)
                    self.cp(ks.t[:], pt.t[:, 0:128], [pt], [ks], eng="dve")
                    S.dma("sp", ktok[i * 128:(i + 1) * 128, (j - 4) * 128:(j - 3) * 128], ks.t[:], reads=[ks])
        S.phase_end()
        if not hasattr(self, "_ml_scal"):
            self._ml_scal = S.sb("scal", [64, nch, 24], F32)
            self._ml_eFbc = S.sb("eFbc", [128, 8, nch], F32)
        scal, eFbc = self._ml_scal, self._ml_eFbc
        S.phase_begin()
        KS = float(np.log(128.0 ** -0.5))
        E = S.sb("Esel", [4, 4, 128], F32)
        for j in range(4):
            self.cp(E.t[:, j, :], self.ident_f.t[0:4, j:j + 1].to_broadcast([4, 128]), [self.ident_f], [E])
        m01 = S.sb("m01", [4, NT], F32)
        S.op("dve", lambda: nc.vector.memset(m01.t[:], 1.0), writes=[m01])
        S.op("dve", lambda: nc.vector.memset(m01.t[:].rearrange("p (c s) -> p c s", s=64)[:, :, 0:1], 0.0), writes=[m01])
        gb = S.sb("gbias", [4, 4], F32)
        for g4 in range(4):
            S.dma("sp", gb.t[:, g4:g4 + 1], self.W["mlstm_gate_b"][l, g4 * 4:(g4 + 1) * 4].rearrange("(p o) -> p o", o=1), writes=[gb])
        I_ = S.sb("gI", [4, NT], F32); L_ = S.sb("gL", [4, NT], F32); PL = S.sb("gPL", [4, NT], F32)
        CUM = S.sb("gCUM", [4, NT], F32); q3 = [S.sb("gq%d" % i, [4, NT], F32) for i in range(3)]
        eF = S.sb("geF", [4, nch], F32)
        for dr in range(2):
            S.dma("sp", I_.t[:], cg[dr * 8:dr * 8 + 4, :], writes=[I_])
            S.dma("sp", L_.t[:], cg[dr * 8 + 4:dr * 8 + 8, :], writes=[L_])
            self.ts(I_.t[:], I_.t[:], gb.t[:, 2 * dr:2 * dr + 1], None, ALU.add, None, [I_, gb], [I_])
            self.ts(L_.t[:], L_.t[:], gb.t[:, 2 * dr + 1:2 * dr + 2], None, ALU.add, None, [L_, gb], [L_])
            self.act(L_.t[:], L_.t[:], AF.Exp, [L_], [L_], scale=-1.0)
            self.act(L_.t[:], L_.t[:], AF.Ln, [L_], [L_], bias=1.0)
            S.op("dve", lambda: nc.vector.tensor_tensor_scan(PL.t[:], m01.t[:], L_.t[:], 0.0, ALU.mult, ALU.add), [m01, L_], [PL])
            PLv = PL.t[:].rearrange("p (c s) -> p c s", s=64)
            PLlast = PLv[:, :, 63:64].broadcast_to([4, nch, 64])
            if dr == 0:
                self.cp(CUM.t[:], PL.t[:], [PL], [CUM])
            else:
                self.tt(CUM.t[:], L_.t[:], PL.t[:], ALU.subtract, [L_, PL], [CUM])
                self.tt(CUM.t[:].rearrange("p (c s) -> p c s", s=64), CUM.t[:].rearrange("p (c s) -> p c s", s=64), PLlast,
                        ALU.add, [CUM, PL], [CUM])
            self.tt(q3[0].t[:], I_.t[:], CUM.t[:], ALU.add, [I_, CUM], [q3[0]])
            self.tt(q3[1].t[:].rearrange("p (c s) -> p c s", s=64), q3[0].t[:].rearrange("p (c s) -> p c s", s=64), PLlast,
                    ALU.subtract, [q3[0], PL], [q3[1]])
            kb_ = S.sb("ksb%d" % dr, [4, 1], F32)
            S.op("dve", lambda: nc.vector.memset(kb_.t[:], KS), writes=[kb_])
            self.act(q3[0].t[:], q3[0].t[:], AF.Exp, [q3[0], kb_], [q3[0]], bias=kb_.t[:, 0:1])
            self.act(q3[1].t[:], q3[1].t[:], AF.Exp, [q3[1], kb_], [q3[1]], bias=kb_.t[:, 0:1])
            self.act(q3[2].t[:], CUM.t[:], AF.Exp, [CUM], [q3[2]])
            self.act(eF.t[:], PLv[:, :, 63], AF.Exp, [PL], [eF], scale=-1.0)
            for c in range(nch):
                ps = self.rr("pb", self.pb)
                for q in range(3):
                    self.mm(ps.t[0:64, q * 4:(q + 1) * 4], q3[q].t[:, c * 64:(c + 1) * 64], self.ident_f.t[0:4, 0:4], True, True,
                            [q3[q], self.ident_f], [ps])
                self.cp(scal.t[:, c, dr * 12:(dr + 1) * 12], ps.t[0:64, 0:12], [ps], [scal], eng=("act" if c % 2 else "dve"))
            for j in range(4):
                ps = self.rr("pb", self.pb)
                self.mm(ps.t[:, 0:nch], E.t[:, j, :], eF.t[:], True, True, [E, eF], [ps])
                self.cp(eFbc.t[:, dr * 4 + j, :], ps.t[:, 0:nch], [ps], [eFbc], eng="act")
        if cfg.debug:
            S.dma("sp", d("dbg_scal", [64, nch, 24], F32), scal.t[:], reads=[scal])
            S.dma("sp", d("dbg_eF", [128, 8, nch], F32), eFbc.t[:], reads=[eFbc])
        S.phase_end()
        S.phase_begin()
        maskt = S.sb("maskt", [64, 2, 64], F32)
        S.dma("sp", maskt.t[:], self.mlmask, writes=[maskt])
        order = {0: list(range(nch)), 1: list(range(cch - 1, -1, -1)) + list(range(nch - 1, cch - 1, -1))}
        qTb = [S.sb("mqT%d" % i, [128, NT], BF16) for i in range(2)]
        kTb = [S.sb("mkT%d" % i, [128, NT], BF16) for i in range(2)]
        kTokb = [S.sb("mktok%d" % i, [64, nch, 128], BF16) for i in range(2)]
        Vxb = [S.sb("mvx%d" % i, [64, nch, 129], BF16) for i in range(2)]
        st_f = [S.sb("mst%d" % i, [128, 129], F32) for i in range(4)]
        st_b = [S.sb("mstb%d" % i, [128, 129], BF16) for i in range(4)]
        Sm2 = [[S.sb("mSm%d_%d" % (i, k), [64, 64], BF16) for k in range(2)] for i in range(4)]
        Vwb = [S.sb("mVw%d" % i, [64, nch, 129], BF16) for i in range(4)]
        dn = [S.sb("mdn%d" % i, [64, 2], F32) for i in range(4)]
        hst = [[S.sb("mh%d_%d" % (i, k), [64, 128], F32) for k in range(2)] for i in range(4)]
        for hp in range(2):
            heads = (2 * hp, 2 * hp + 1)
            for i, j in enumerate(heads):
                S.dma("sp", qTb[i].t[:], cqT[j * 128:(j + 1) * 128, :], writes=[qTb[i]])
                S.dma("sp", kTb[i].t[:], cqT[512 + j * 128:512 + (j + 1) * 128, :], writes=[kTb[i]])
                S.dma("sp", kTokb[i].t[:], ktok[:, j * 128:(j + 1) * 128].rearrange("(c p) d -> p c d", p=64), writes=[kTokb[i]])
                S.dma("sp", Vxb[i].t[:, :, 0:128], cv[:, j * 128:(j + 1) * 128].rearrange("(c p) d -> p c d", p=64), writes=[Vxb[i]])
                S.op("dve", lambda: nc.vector.memset(Vxb[i].t[:, :, 128:129], 1.0), writes=[Vxb[i]])
            chains = [(i, j, dr) for i, j in enumerate(heads) for dr in range(2)]
            for ci, (i, j, dr) in enumerate(chains):
                cw_ = dr * 12 + 4 + j
                self.tt(Vwb[ci].t[:], Vxb[i].t[:], scal.t[:, :, cw_:cw_ + 1].broadcast_to([64, nch, 129]), ALU.mult,
                        [Vxb[i], scal], [Vwb[ci]])
                S.op("dve", lambda: nc.vector.memset(st_f[ci].t[:], 0.0), writes=[st_f[ci]])
                S.op("dve", lambda: nc.vector.memset(st_b[ci].t[:], 0.0), writes=[st_b[ci]])
            ubank = [self.bank[6], self.bank[7]]

            def step_info(step):
                out_ = []
                for ci, (i, j, dr) in enumerate(chains):
                    c = order[dr][step]
                    out_.append((ci, i, j, dr, c, slice(c * 64, (c + 1) * 64), dr * 12 + j))
                return out_

            def scores(step):
                sbk = self.bank[step % 2]
                for (ci, i, j, dr, c, cs, cb) in step_info(step):
                    self.mm(sbk.t[0:64, ci * 64:(ci + 1) * 64], kTb[i].t[:, cs], qTb[i].t[:, cs], True, True, [kTb[i], qTb[i]], [sbk])
                for (ci, i, j, dr, c, cs, cb) in step_info(step):
                    sm = Sm2[ci][step % 2]
                    self.stt(sm.t[:], sbk.t[0:64, ci * 64:(ci + 1) * 64], scal.t[:, c, cb:cb + 1], maskt.t[:, dr, :], ALU.mult, ALU.mult,
                             [sbk, scal, maskt], [sm])
            scores(0)
            for step in range(nch):
                info = step_info(step)
                Sm = [Sm2[ci][step % 2] for ci in range(4)]
                if step + 1 < nch:
                    scores(step + 1)
                for (ci, i, j, dr, c, cs, cb) in info:
                    ps_o = self.bank[2 + ci]
                    self.mm(ps_o.t[0:64, 0:129], qTb[i].t[:, cs], st_b[ci].t[:], True, False, [qTb[i], st_b[ci]], [ps_o])
                    self.mm(ps_o.t[0:64, 0:129], Sm[ci].t[:], Vxb[i].t[:, c, :], False, True, [Sm[ci], Vxb[i]], [ps_o])
                for (ci, i, j, dr, c, cs, cb) in info:
                    ub = ubank[ci // 2]
                    uo = (ci % 2) * 256
                    self.mm(ub.t[:, uo:uo + 129], kTokb[i].t[:, c, :], Vwb[ci].t[:, c, :], True, True, [kTokb[i], Vwb[ci]], [ub])
                for (ci, i, j, dr, c, cs, cb) in info:
                    ub = ubank[ci // 2]
                    uo = (ci % 2) * 256
                    self.stt(st_f[ci].t[:], st_f[ci].t[:], eFbc.t[:, dr * 4 + j, c:c + 1], ub.t[:, uo:uo + 129], ALU.mult, ALU.add,
                             [st_f[ci], eFbc, ub], [st_f[ci]])
                for (ci, i, j, dr, c, cs, cb) in info:
                    self.ts(dn[ci].t[:, 0:1], self.bank[2 + ci].t[0:64, 128:129], -1.0, scal.t[:, c, cb + 8:cb + 9], ALU.mult, ALU.max,
                            [self.bank[2 + ci], scal], [dn[ci]])
                for (ci, i, j, dr, c, cs, cb) in info:
                    self.tt(dn[ci].t[:, 0:1], dn[ci].t[:, 0:1], self.bank[2 + ci].t[0:64, 128:129], ALU.max, [dn[ci], self.bank[2 + ci]], [dn[ci]])
                for (ci, i, j, dr, c, cs, cb) in info:
                    self.recip(dn[ci].t[:, 1:2], dn[ci].t[:, 0:1], [dn[ci]], [dn[ci]])
                for (ci, i, j, dr, c, cs, cb) in info:
                    self.cp(st_b[ci].t[:], st_f[ci].t[:], [st_f[ci]], [st_b[ci]], eng="act")
                for (ci, i, j, dr, c, cs, cb) in info:
                    ps_o = self.bank[2 + ci]
                    h = self.rr("mh%d" % ci, hst[ci])
                    self.act(h.t[:], ps_o.t[0:64, 0:128], AF.Copy, [ps_o, dn[ci]], [h], scale=dn[ci].t[:, 1:2])
                    if not (last and c < cch):
                        S.dma("sp", (hf if dr == 0 else hb)[cs, j * 128:(j + 1) * 128], h.t[:], reads=[h])
        S.phase_end()
        S.phase_begin()
        ng = S.sb("mng", [128, 128], F32)
        self.load_bc(ng, self.W["mlstm_norm_g"][l])
        ha = [S.sb("mha%d" % i, [128, 512], F32) for i in range(2)]
        hb_ = [S.sb("mhb%d" % i, [128, 512], F32) for i in range(2)]
        og = [S.sb("mog%d" % i, [128, 512], BF16) for i in range(2)]
        sqs = [S.sb("msq%d" % i, [128, 512], F32) for i in range(2)]
        ss = [S.sb("mss%d" % i, [128, 4], F32) for i in range(2)]
        mo = [S.sb("mmo%d" % i, [128, 512], BF16) for i in range(2)]
        stT = [S.sb("mstT%d" % i, [128, 4, 512], BF16) for i in range(2)]
        for (u0, n, isc) in cfg.blocks:
            if isc and last:
                continue
            stg = self.rr("mstT", stT)
            jobs = []
            for jt in range(n // 128):
                def job(jt=jt, u=u0 + jt * 128, stg=stg):
                    a = self.rr("mha", ha); b = self.rr("mhb", hb_); o = self.rr("mog", og); s_ = self.rr("mss", ss); m = self.rr("mmo", mo)
                    sq = self.rr("msq", sqs); pt = self.rr("pt", self.pt)
                    av3 = a.t[:].rearrange("p (h d) -> p h d", h=4)

                    def loads():
                        S.dma("pool", a.t[:], hf[u:u + 128, :], writes=[a])
                        S.dma("pool", b.t[:], hb[u:u + 128, :], writes=[b])
                        S.dma("pool", o.t[:], cog[u:u + 128, :], writes=[o])

                    def trs():
                        for b4 in range(4):
                            self.tr(pt.t[:, b4 * 128:(b4 + 1) * 128], m.t[:, b4 * 128:(b4 + 1) * 128], self.ident_b.t[:], [m, self.ident_b], [pt],
                                    inc=(b4 == 3))
                    sts = [loads,
                           lambda: self.tt(a.t[:], a.t[:], b.t[:], ALU.add, [a, b], [a]),
                           lambda: self.act(sq.t[:], a.t[:], AF.Square, [a], [sq]),
                           lambda: S.op("dve", lambda: nc.vector.tensor_reduce(s_.t[:], sq.t[:].rearrange("p (h d) -> p h d", h=4), AX.X, ALU.add), [sq], [s_])]
                    sts += self.rstd_stages(s_.t[:], s_.t[:], 128, s_)
                    sts += [lambda: self.tt(av3, av3, s_.t[:].unsqueeze(2).broadcast_to([128, 4, 128]), ALU.mult, [a, s_], [a]),
                            lambda: self.tt(av3, av3, ng.t[:].unsqueeze(1).broadcast_to([128, 4, 128]), ALU.mult, [a, ng], [a]),
                            lambda: self.tt(m.t[:], a.t[:], o.t[:], ALU.mult, [a, o], [m]),
                            trs,
                            lambda: self.cp(stg.t[:, :, jt * 128:(jt + 1) * 128], pt.t[:, 0:512].rearrange("p (b t) -> p b t", b=4), [pt], [stg], eng="act")]
                    return sts
                jobs.append(job)
            for g0 in range(0, len(jobs), 2):
                self.emit_interleaved([jb() for jb in jobs[g0:g0 + 2]], 2)
            S.dma("sp", mT[:, u0:u0 + n].rearrange("(b p) t -> p b t", p=128), stg.t[:, :, 0:n], reads=[stg])
        S.phase_end()

    def phase_merge(self, l):
        S, nc, cfg = self.S, self.nc, self.cfg
        T, TC, NT = cfg.T, cfg.TC, cfg.NT
        d = self.dscr
        last = (l == DEPTH - 1)
        moe = (l % 2 == 1)
        brT = [d("aT", [512, NT], BF16), d("bT", [512, NT], BF16), d("mT", [512, NT], BF16)]
        gT = d("gT", [3072, NT], BF16)
        xmid = d("xmid", [NT, DM], F32)
        xn2T = d("xn2T", [DM, NT], BF16)
        gates = d("gates", [NT, N_EXP], F32)
        S.phase_begin()
        wbr = []
        for i, nm in enumerate(("w_br_attn", "w_br_diff", "w_br_mlstm")):
            w = S.sb("wbr%d" % i, [128, 4, DM], BF16)
            S.dma("pool", w.t[:], self.W[nm][l].rearrange("(k p) n -> p k n", p=128), writes=[w])
            wbr.append(w)
        wo = S.sb("wo", [128, KT, DM], BF16)
        S.dma("pool", wo.t[:], self.W["w_out"][l].rearrange("(k p) n -> p k n", p=128), writes=[wo])
        mods = {}
        tmps = [S.sb("dtmp%d" % k, [128, DM], F32) for k in range(1)]
        gsh = tmps[0]
        for which in ((0,) if last else (1, 0)):
            G1 = S.sb("G1_%d" % which, [128, DM], F32)
            self.load_bc(G1, self.mod[l, which, 2 * DM:3 * DM]); self.load_bc(gsh, self.W["post_mix_g"][l])
            self.tt(G1.t[:], G1.t[:], gsh.t[:], ALU.mult, [G1, gsh], [G1])
            A2, SH2 = self.make_mod_tiles(l, "pre_ffn_g", 4, 3, which, "d%d" % which, gbuf=gsh)
            mods[which] = (G1, A2, SH2)
        if moe:
            wr = S.sb("wr", [128, KT, N_EXP], F32)
            S.dma("sp", wr.t[:], self.W["w_router"][0].rearrange("(k p) e -> p k e", p=128), writes=[wr])
            br_ = S.sb("brt", [128, N_EXP], F32)
            self.load_bc(br_, self.W["b_router"][0])
            xnfs = [S.sb("xnf%d" % k, [128, DM], F32) for k in range(2)]; xnfT = S.sb("xnfT", [128, KT, 128], F32)
            lgs = [S.sb("lg%d" % k, [128, 8], F32) for k in range(2)]; mxs = [S.sb("mx8_%d" % k, [128, 8], F32) for k in range(2)]
            wvs = [S.sb("wv%d" % k, [128, 4], F32) for k in range(2)]
            m1s = [S.sb("gm1_%d" % k, [128, 8], F32) for k in range(2)]; m2s = [S.sb("gm2_%d" % k, [128, 8], F32) for k in range(2)]
        brb = [[S.sb("brb%d_%d" % (i, k), [128, 4, 512], BF16) for k in range(2)] for i in range(3)]
        gb = [S.sb("gtb%d" % k, [128, 24, 512], BF16) for k in range(2)]
        zT = [S.sb("zT%d" % k, [128, KT, 512], BF16) for k in range(2)]
        t1 = [S.sb("dt1_%d" % k, [128, 512], F32) for k in range(2)]
        t2 = [S.sb("dt2_%d" % k, [128, 512], F32) for k in range(2)]
        xt = [S.sb("dxt%d" % k, [128, DM], F32) for k in range(2)]
        x1 = [S.sb("dx1%d" % k, [128, DM], F32) for k in range(2)]
        junks = [S.sb("djunk%d" % k, [128, 512], F32) for k in range(1)]
        st = [S.sb("dst%d" % k, [128, 4], F32) for k in range(2)]
        st2 = [S.sb("dst2%d" % k, [128, 2], F32) for k in range(2)]
        xb = [S.sb("dxb%d" % k, [128, DM], BF16) for k in range(2)]
        stT = [S.sb("dstT%d" % k, [128, KT, 512], BF16) for k in range(1)]
        pend = []
        for (u0, n, isc) in cfg.blocks:
            if isc and last:
                continue
            G1, A2, SH2 = mods[1 if isc else 0]
            bt = []
            for i in range(3):
                b = self.rr("brb%d" % i, brb[i])
                S.dma("pool", b.t[:, :, 0:n], brT[i][:, u0:u0 + n].rearrange("(k p) t -> p k t", p=128), writes=[b])
                bt.append(b)
            g = self.rr("gtb", gb)
            S.dma("pool", g.t[:, :, 0:n], gT[:, u0:u0 + n].rearrange("(k p) t -> p k t", p=128), writes=[g])
            z = self.rr("zT", zT)
            for ft in range(8):
                pss = []
                for i in range(3):
                    ps = self.rr("pb", self.pb)
                    for kt in range(4):
                        self.mm(ps.t[:, 0:n], wbr[i].t[:, kt, ft * 128:(ft + 1) * 128], bt[i].t[:, kt, 0:n], kt == 0, kt == 3,
                                [wbr[i], bt[i]], [ps])
                    pss.append(ps)
                a1 = self.rr("dt1", t1); a2 = self.rr("dt2", t2)
                self.tt(a1.t[:, 0:n], pss[0].t[:, 0:n], g.t[:, 0 + ft, 0:n], ALU.mult, [pss[0], g], [a1])
                self.tt(a2.t[:, 0:n], pss[1].t[:, 0:n], g.t[:, 8 + ft, 0:n], ALU.mult, [pss[1], g], [a2])
                self.tt(a1.t[:, 0:n], a1.t[:, 0:n], a2.t[:, 0:n], ALU.add, [a1, a2], [a1], eng="pool")
                self.tt(a2.t[:, 0:n], pss[2].t[:, 0:n], g.t[:, 16 + ft, 0:n], ALU.mult, [pss[2], g], [a2])
                self.tt(z.t[:, ft, 0:n], a1.t[:, 0:n], a2.t[:, 0:n], ALU.add, [a1, a2], [z], eng="pool")
            stg = self.rr("dstT", stT)
            for j in range(n // 128):
                u = u0 + j * 128
                x = self.rr("dxt", xt); xo = self.rr("dx1", x1); s_ = self.rr("dst", st); s2 = self.rr("dst2", st2); xb_ = self.rr("dxb", xb)
                S.dma("pool", x.t[:], self.x_src(l, u), writes=[x])
                pp = []
                tmp = self.rr("dtmp", tmps)
                for ch in range(2):
                    junk = self.rr("djunk", junks)
                    ps = self.rr("pb", self.pb)
                    for kt in range(KT):
                        self.mm(ps.t[:, :], z.t[:, kt, j * 128:(j + 1) * 128], wo.t[:, kt, ch * 512:(ch + 1) * 512], kt == 0, kt == KT - 1,
                                [z, wo], [ps])
                    self.act(junk.t[:], ps.t[:], AF.Square, [ps], [junk, s_], accum_out=s_.t[:, ch:ch + 1])
                    pp.append(ps)
                self.tt(s_.t[:, 2:3], s_.t[:, 0:1], s_.t[:, 1:2], ALU.add, [s_], [s_])
                self.rstd(s_.t[:, 3:4], s_.t[:, 2:3], DM, s_)
                for ch in range(2):
                    cs = slice(ch * 512, (ch + 1) * 512)
                    self.stt(tmp.t[:, cs], pp[ch].t[:], s_.t[:, 3:4], G1.t[:, cs], ALU.mult, ALU.mult, [pp[ch], s_, G1], [tmp])
                self.tt(xo.t[:], tmp.t[:], x.t[:], ALU.add, [tmp, x], [xo])
                S.dma("sp", xmid[u:u + 128, :], xo.t[:], reads=[xo])
                if moe:
                    xnf = self.rr("xnf", xnfs)
                    self.rms_mod(xo, A2, SH2, xnf, tmp, s2)
                    self.cp(xb_.t[:], xnf.t[:], [xnf], [xb_], eng="act")

                def router(xnf=(xnf if moe else None), u=u):
                    lg = self.rr("lg", lgs); mx = self.rr("mx8", mxs); wv = self.rr("wv", wvs); m1 = self.rr("gm1", m1s); m2 = self.rr("gm2", m2s)
                    for kt in range(KT):
                        ps = self.rr("pb", self.pb)
                        self.tr(ps.t[:, 0:128], xnf.t[:, kt * 128:(kt + 1) * 128], self.ident_f.t[:], [xnf, self.ident_f], [ps])
                        self.cp(xnfT.t[:, kt, :], ps.t[:, 0:128], [ps], [xnfT], eng=("act" if kt % 2 else "dve"))
                    ps = self.rr("pb", self.pb)
                    for kt in range(KT):
                        self.mm(ps.t[:, 0:N_EXP], xnfT.t[:, kt, :], wr.t[:, kt, :], kt == 0, kt == KT - 1, [xnfT, wr], [ps])
                    self.tt(lg.t[:], ps.t[:, 0:N_EXP], br_.t[:], ALU.add, [ps, br_], [lg])
                    S.op("dve", lambda: nc.vector.max(mx.t[:], lg.t[:]), [lg], [mx])
                    self.tt(wv.t[:, 0:1], mx.t[:, 1:2], mx.t[:, 0:1], ALU.subtract, [mx], [wv])
                    self.act(wv.t[:, 0:1], wv.t[:, 0:1], AF.Exp, [wv], [wv])
                    self.ts(wv.t[:, 0:1], wv.t[:, 0:1], 1.0, None, ALU.add, None, [wv], [wv])
                    self.recip(wv.t[:, 1:2], wv.t[:, 0:1], [wv], [wv])
                    self.ts(wv.t[:, 2:3], wv.t[:, 1:2], -1.0, 1.0, ALU.mult, ALU.add, [wv], [wv])
                    self.ts(m1.t[:], lg.t[:], mx.t[:, 0:1], wv.t[:, 1:2], ALU.is_equal, ALU.mult, [lg, mx, wv], [m1])
                    self.ts(m2.t[:], lg.t[:], mx.t[:, 1:2], wv.t[:, 2:3], ALU.is_equal, ALU.mult, [lg, mx, wv], [m2])
                    self.tt(m1.t[:], m1.t[:], m2.t[:], ALU.add, [m1, m2], [m1])
                    S.dma("sp", gates[u:u + 128, :], m1.t[:], reads=[m1])
                if not moe:
                    self.rms_mod(xo, A2, SH2, xb_, tmp, s2)

                def tail(xb_=xb_, stg=stg, j=j, u0=u0, n=n, router=router):
                    if moe:
                        router()
                    pt = self.rr("pt", self.pt)
                    for kt in range(KT):
                        self.tr(pt.t[:, kt * 128:(kt + 1) * 128], xb_.t[:, kt * 128:(kt + 1) * 128], self.ident_b.t[:], [xb_, self.ident_b], [pt],
                                inc=(kt == KT - 1))
                    self.cp(stg.t[:, :, j * 128:(j + 1) * 128], pt.t[:].rearrange("p (k t) -> p k t", k=KT), [pt], [stg], eng="act")
                    if j == n // 128 - 1:
                        S.dma("sp", xn2T[:, u0:u0 + n].rearrange("(k p) t -> p k t", p=128), stg.t[:, :, 0:n], reads=[stg])
                pend.append(tail)
                while len(pend) > 1:
                    pend.pop(0)()
        while pend:
            pend.pop(0)()
        S.phase_end()

    def phase_ffn(self, l):
        S, nc, cfg = self.S, self.nc, self.cfg
        T, TC, NT = cfg.T, cfg.TC, cfg.NT
        d = self.dscr
        last = (l == DEPTH - 1)
        moe = (l % 2 == 1)
        j_ = l // 2
        xmid = d("xmid", [NT, DM], F32); xn2T = d("xn2T", [DM, NT], BF16); gates = d("gates", [NT, N_EXP], F32)
        facc = d("facc", [NT, DM], F32)
        units = []
        if not moe:
            for (h0, nh) in ((0, 6), (6, 6), (12, 5), (17, 5)):
                units.append((self.W["w_ff_gate"][j_], self.W["w_ff_up"][j_], self.W["w_ff_down"][j_], h0, nh, None))
        else:
            for e in range(N_EXP):
                for (h0, nh) in ((0, 6), (6, 5)):
                    units.append((self.W["w_moe_gate"][j_, e], self.W["w_moe_up"][j_, e], self.W["w_moe_down"][j_, e], h0, nh, e))
        S.phase_begin()
        wgb = [S.sb("wg%d" % k, [128, KT, 768], BF16) for k in range(2)]
        wub = [S.sb("wu%d" % k, [128, KT, 768], BF16) for k in range(2)]
        wdb = [S.sb("wd%d" % k, [128, 6, DM], BF16) for k in range(2)]
        xb = [S.sb("exn%d" % k, [128, KT, 512], BF16) for k in range(2)]
        hT = [S.sb("ehT%d" % k, [128, 6, 512], BF16) for k in range(2)]
        sg = [S.sb("esg%d" % k, [128, 512], F32) for k in range(2)]
        ob = [S.sb("eo%d" % k, [128, DM], F32) for k in range(3)]
        gtl = [S.sb("egt%d" % k, [128, 4, N_EXP], F32) for k in range(2)]
        facc_b = {}
        blocks = [b for b in cfg.blocks if not (b[2] and last)]
        for ui, (wg_ap, wu_ap, wd_ap, h0, nh, ex) in enumerate(units):
            wg = self.rr("wg", wgb); wu = self.rr("wu", wub); wd = self.rr("wd", wdb)
            S.dma("pool", wg.t[:, :, 0:nh * 128], wg_ap.rearrange("(k p) n -> p k n", p=128)[:, :, h0 * 128:(h0 + nh) * 128], writes=[wg])
            S.dma("pool", wu.t[:, :, 0:nh * 128], wu_ap.rearrange("(k p) n -> p k n", p=128)[:, :, h0 * 128:(h0 + nh) * 128], writes=[wu])
            S.dma("pool", wd.t[:, 0:nh, :], wd_ap[h0 * 128:(h0 + nh) * 128, :].rearrange("(k p) n -> p k n", p=128), writes=[wd])
            for (u0, n, isc) in blocks:
                x = self.rr("exn", xb)
                S.dma("sp", x.t[:, :, 0:n], xn2T[:, u0:u0 + n].rearrange("(k p) t -> p k t", p=128), writes=[x])
                if ex is not None:
                    gt = self.rr("egt", gtl)
                    S.dma("sp", gt.t[:, 0:n // 128, :], gates[u0:u0 + n, :].rearrange("(j p) e -> p j e", p=128), writes=[gt])
                h = self.rr("ehT", hT)
                for ht in range(nh):
                    pg = self.rr("e_pg", [self.pb[0], self.pb[1]]); pu = self.rr("e_pu", [self.pb[2], self.pb[3]])
                    for kt in range(KT):
                        self.mm(pg.t[:, 0:n], wg.t[:, kt, ht * 128:(ht + 1) * 128], x.t[:, kt, 0:n], kt == 0, kt == KT - 1, [wg, x], [pg])
                    for kt in range(KT):
                        self.mm(pu.t[:, 0:n], wu.t[:, kt, ht * 128:(ht + 1) * 128], x.t[:, kt, 0:n], kt == 0, kt == KT - 1, [wu, x], [pu])
                    s_ = self.rr("esg", sg)
                    self.act(s_.t[:, 0:n], pg.t[:, 0:n], AF.Silu, [pg], [s_])
                    self.tt(h.t[:, ht, 0:n], s_.t[:, 0:n], pu.t[:, 0:n], ALU.mult, [s_, pu], [h])
                for j in range(n // 128):
                    u = u0 + j * 128
                    o = self.rr("eo", ob)
                    for ch in range(2):
                        ps = self.rr("e_pd", [self.pb[4], self.pb[5]])
                        for ht in range(nh):
                            self.mm(ps.t[:, :], h.t[:, ht, j * 128:(j + 1) * 128], wd.t[:, ht, ch * 512:(ch + 1) * 512], ht == 0, ht == nh - 1,
                                    [h, wd], [ps])
                        if ex is None:
                            self.cp(o.t[:, ch * 512:(ch + 1) * 512], ps.t[:], [ps], [o], eng=("act" if ch else "dve"))
                        else:
                            self.act(o.t[:, ch * 512:(ch + 1) * 512], ps.t[:], AF.Copy, [ps, gt], [o], scale=gt.t[:, j, ex:ex + 1])
                    fb = facc_b.setdefault(u, Buf("facc%d" % u))
                    if ui == 0:
                        S.dma("pool", facc[u:u + 128, :], o.t[:], reads=[o], writes=[fb])
                    else:
                        S.dma("pool", facc[u:u + 128, :], o.t[:], reads=[o], writes=[fb], accum_op=ALU.add)
        S.phase_end()
        S.phase_begin()
        G2 = {}
        for which in ((0,) if last else (1, 0)):
            G = S.sb("G2_%d" % which, [128, DM], F32); g = S.sb("gpf_%d" % which, [128, DM], F32)
            self.load_bc(G, self.mod[l, which, 5 * DM:6 * DM]); self.load_bc(g, self.W["post_ffn_g"][l])
            self.tt(G.t[:], G.t[:], g.t[:], ALU.mult, [G, g], [G])
            G2[which] = G
        fa = [S.sb("fa%d" % k, [128, DM], F32) for k in range(2)]
        xm = [S.sb("fxm%d" % k, [128, DM], F32) for k in range(2)]
        xo = [S.sb("fxo%d" % k, [128, DM], F32) for k in range(2)]
        ftmp = [S.sb("ftmp%d" % k, [128, DM], F32) for k in range(2)]
        st = [S.sb("fst%d" % k, [128, 2], F32) for k in range(2)]
        jobs = []
        for (u0, n, isc) in blocks:
            for j in range(n // 128):
                def job(u=u0 + j * 128, isc=isc):
                    f = self.rr("fa", fa); x = self.rr("fxm", xm); o = self.rr("fxo", xo); s_ = self.rr("fst", st); tmp = self.rr("ftmp", ftmp)
                    if last:
                        dst = self.out[u - TC:u - TC + 128, :]
                    elif isc:
                        dst = self.cres[u:u + 128, :]
                    else:
                        dst = self.xres[u - TC:u - TC + 128, :]

                    def loads():
                        S.dma("pool", f.t[:], facc[u:u + 128, :], writes=[f])
                        S.dma("pool", x.t[:], xmid[u:u + 128, :], writes=[x])
                    sts = [loads]
                    sts += self.rms_mod_stages(f, G2[1 if isc else 0], None, tmp, tmp, s_)
                    sts.append(lambda: self.tt(o.t[:], tmp.t[:], x.t[:], ALU.add, [tmp, x], [o]))
                    sts.append(lambda: S.dma("sp", dst, o.t[:], reads=[o]))
                    return sts
                jobs.append(job)
        for g0 in range(0, len(jobs), 2):
            self.emit_interleaved([jb() for jb in jobs[g0:g0 + 2]], 2)
        S.phase_end()


def rope_tables(T):
    GRID_W = 64
    t = np.arange(T)
    row = (t // GRID_W).astype(np.float32)
    col = (t % GRID_W).astype(np.float32)
    inv = (10000.0 ** (-np.arange(16, dtype=np.float32) / 16)).astype(np.float32)
    ar = row[:, None] * inv
    ac = col[:, None] * inv
    C = np.concatenate([np.cos(ar), np.cos(ar), np.cos(ac), np.cos(ac)], axis=1).astype(np.float32)
    Sn = np.concatenate([-np.sin(ar), np.sin(ar), -np.sin(ac), np.sin(ac)], axis=1).astype(np.float32)
    return C, Sn


def build_program(cfg):
    B = Builder(cfg)
    B.phase_mod()
    for l in range(DEPTH):
        B.phase_A(l)
        if cfg.stop_after == "A":
            break
        B.phase_attn(l, "a")
        B.phase_attn(l, "b")
        if cfg.stop_after == "B":
            break
        B.phase_mlstm(l)
        if cfg.stop_after == "C":
            break
        B.phase_merge(l)
        if cfg.stop_after == "D":
            break
        B.phase_ffn(l)
        if cfg.stop_after == "E":
            break
    B.S.finish()
    return B


def make_in_maps(inputs, cfg, n_cores):
    f = lambda a: np.ascontiguousarray(np.asarray(a, dtype=np.float32))
    C, Sn = rope_tables(cfg.T)
    shared = {k: f(v) for k, v in inputs.items() if k not in ("x", "c", "ctx", "c_ctx")}
    shared["ropeC"] = C
    shared["ropeS"] = Sn
    shared["ident"] = np.eye(128, dtype=np.float32)
    s_ = np.arange(64)[:, None]; t_ = np.arange(64)[None, :]
    shared["mlmask"] = np.ascontiguousarray(np.stack([(s_ <= t_), (s_ >= t_)], axis=1).astype(np.float32))
    shared["cctxT"] = f(np.asarray(inputs["c_ctx"]).reshape(KT, 128).T)
    maps = []
    for b in range(n_cores):
        m = dict(shared)
        m["x"] = f(inputs["x"][b])
        m["ctx"] = f(inputs["ctx"][b])
        m["cT"] = f(np.asarray(inputs["c"][b]).reshape(KT, 128).T)
        maps.append(m)
    return maps


def kernel(**inputs):
    cfg = Cfg()
    B = build_program(cfg)
    maps = make_in_maps(inputs, cfg, 8)
    res = run_bass_kernel_spmd(B.nc, maps, core_ids=list(range(8)))
    return np.stack([np.asarray(r["out"]) for r in res.results], axis=0).astype(np.float32)
```
